# Optimizing a Trainium2 kernel written in Bass

```python
import math
import jax, jax.numpy as jnp
from jax import lax
import numpy as np

D_MODEL = 2048
BATCH = 2
SEQ = 8192
DEPTH = 4

GRID_W = 64
CTX_LEN = 256
N_MOD = 6
ATT_HEADS = 8
ATT_HEAD_DIM = 64
ATT_V_DIM = 2 * ATT_HEAD_DIM
QK_WIDTH = ATT_HEADS * 2 * ATT_HEAD_DIM
ATT_WIDTH = ATT_HEADS * ATT_V_DIM
Q_BLOCK = 128
ROPE_BASE = 10000.0
AXIS_ROT = ATT_HEAD_DIM // 2
POOL_WINDOWS = (2, 4, 8, 16)
POOL_GROUPS = len(POOL_WINDOWS)
POOL_WIDTH = D_MODEL // 4
POOL_GROUP_DIM = POOL_WIDTH // POOL_GROUPS
CONV_WIDTH = D_MODEL // 4
CONV_K = 3
MIX_WIDTH = ATT_WIDTH + POOL_WIDTH + CONV_WIDTH
IN_WIDTH = 2 * QK_WIDTH + ATT_WIDTH + POOL_WIDTH + 3 * CONV_WIDTH
SPLIT_POINTS = (QK_WIDTH, 2 * QK_WIDTH, 2 * QK_WIDTH + ATT_WIDTH,
                2 * QK_WIDTH + ATT_WIDTH + POOL_WIDTH,
                2 * QK_WIDTH + ATT_WIDTH + POOL_WIDTH + CONV_WIDTH,
                2 * QK_WIDTH + ATT_WIDTH + POOL_WIDTH + 2 * CONV_WIDTH)
N_EXPERTS = 16
EXPERT_FF = 1024
CAP_FACTOR = 2
EPS = 1e-6

kernel_name = 'hybrid_parallel_group_dit_block'


def rmsnorm(x, g):
    x32 = x.astype(jnp.float32)
    y = x32 * lax.rsqrt(jnp.mean(x32 * x32, axis=-1, keepdims=True) + EPS)
    return y.astype(x.dtype) * g


def modulate(h, shift, scale):
    return h * (1.0 + scale) + shift


def axial_rope_tables(row, col, dtype):
    inv = 1.0 / (ROPE_BASE ** (jnp.arange(0, AXIS_ROT, 2, dtype=jnp.float32) / AXIS_ROT))
    ang = jnp.concatenate([row[:, None].astype(jnp.float32) * inv,
                           col[:, None].astype(jnp.float32) * inv], axis=-1)
    return jnp.cos(ang).astype(dtype), jnp.sin(ang).astype(dtype)


def apply_rope(x, cos, sin):
    c = cos[None, :, None, None, :]
    s = sin[None, :, None, None, :]
    x1, x2 = x[..., :ATT_HEAD_DIM // 2], x[..., ATT_HEAD_DIM // 2:]
    return jnp.concatenate([x1 * c - x2 * s, x1 * s + x2 * c], axis=-1)


def split_groups(u):
    B, n, _ = u.shape
    q, k, v, p, gb, gc, xin = jnp.split(u, SPLIT_POINTS, axis=-1)
    q = q.reshape(B, n, ATT_HEADS, 2, ATT_HEAD_DIM)
    k = k.reshape(B, n, ATT_HEADS, 2, ATT_HEAD_DIM)
    v = v.reshape(B, n, ATT_HEADS, ATT_V_DIM)
    return q, k, v, p, gb, gc, xin


def diff_attn_core(q, k, v, lam):
    s = jnp.einsum('bqhmd,bkhmd->bhmqk', q, k).astype(jnp.float32) * (ATT_HEAD_DIM ** -0.5)
    a = jax.nn.softmax(s, axis=-1)
    w = (a[:, :, 0] - lam * a[:, :, 1]).astype(v.dtype)
    return jnp.einsum('bhqk,bkhe->bqhe', w, v)


def diff_attn_blocked(q, k, v, lam):
    B, n = q.shape[:2]
    nb = n // Q_BLOCK
    qb = q.reshape(B, nb, Q_BLOCK, ATT_HEADS, 2, ATT_HEAD_DIM).transpose(1, 0, 2, 3, 4, 5)
    o = lax.map(lambda qblk: diff_attn_core(qblk, k, v, lam), qb)
    return o.transpose(1, 0, 2, 3, 4).reshape(B, n, ATT_HEADS, ATT_V_DIM)


def pool_mix(p, pool_w, pool_scale):
    B, n, _ = p.shape
    p32 = p.astype(jnp.float32)
    cs = jnp.concatenate([jnp.zeros((B, 1, POOL_WIDTH), jnp.float32), jnp.cumsum(p32, axis=1)], axis=1)
    t = jnp.arange(n)
    feats = []
    for g, w in enumerate(POOL_WINDOWS):
        lo = jnp.clip(t - w // 2, 0, n)
        hi = jnp.clip(t + w - w // 2, 0, n)
        sl = slice(g * POOL_GROUP_DIM, (g + 1) * POOL_GROUP_DIM)
        cnt = (hi - lo).astype(jnp.float32)[None, :, None]
        feats.append((cs[:, hi, sl] - cs[:, lo, sl]) / cnt - p32[..., sl])
    f = jnp.stack(feats, axis=2).astype(p.dtype)
    y = jnp.einsum('bngc,gcd->bngd', f, pool_w).reshape(B, n, POOL_WIDTH)
    return y * pool_scale


def conv_mix(gb, gc, xin, conv_w):
    z = gc * xin
    zp = jnp.pad(z, ((0, 0), (1, 1), (0, 0)))
    y = zp[:, :-2] * conv_w[0] + zp[:, 1:-1] * conv_w[1] + zp[:, 2:] * conv_w[2]
    return gb * y


def mixer_output(att, p, gb, gc, xin, subln_g, lam_init, pool_w, pool_scale, conv_w, w_out):
    B, n = att.shape[:2]
    a = (rmsnorm(att, subln_g) * (1.0 - lam_init)).reshape(B, n, ATT_WIDTH)
    y = jnp.concatenate([a, pool_mix(p, pool_w, pool_scale), conv_mix(gb, gc, xin, conv_w)], axis=-1)
    return y @ w_out


def expert_choice_ffn(h, w_router, w_gate, w_up, w_down):
    B, n, D = h.shape
    cap = CAP_FACTOR * n // N_EXPERTS
    aff = jax.nn.softmax((h @ w_router).astype(jnp.float32), axis=-1)
    g, idx = lax.top_k(aff.transpose(0, 2, 1), cap)
    xg = jax.vmap(lambda hb, ib: hb[ib])(h, idx)
    a = jnp.einsum('becd,edf->becf', xg, w_gate)
    u = jnp.einsum('becd,edf->becf', xg, w_up)
    y = jnp.einsum('becf,efd->becd', jax.nn.silu(a) * u, w_down) * g[..., None].astype(h.dtype)
    return jax.vmap(lambda ib, yb: jnp.zeros((n, D), yb.dtype).at[ib.reshape(-1)].add(yb.reshape(-1, D)))(idx, y)


def setup_inputs(seed: int = 0) -> dict:
    key = jax.random.key(seed)
    ks = jax.random.split(key, 24)
    nrm = lambda k, s: jax.random.normal(k, s, jnp.float32)
    D, L = D_MODEL, DEPTH
    return {
        'x': nrm(ks[0], (BATCH, SEQ, D)),
        'c': nrm(ks[1], (BATCH, D)),
        'ctx': nrm(ks[2], (BATCH, CTX_LEN, D)),
        'c_ctx': nrm(ks[3], (D,)),
        'w_ada': nrm(ks[4], (L, D, N_MOD * D)) * (0.5 * D ** -0.5),
        'b_ada': nrm(ks[5], (L, N_MOD * D)) * 0.02,
        'norm1_g': 1.0 + 0.02 * nrm(ks[6], (L, D)),
        'norm2_g': 1.0 + 0.02 * nrm(ks[7], (L, D)),
        'w_in': nrm(ks[8], (L, D, IN_WIDTH)) * D ** -0.5,
        'w_out': nrm(ks[9], (L, MIX_WIDTH, D)) * MIX_WIDTH ** -0.5,
        'lambda_q1': 0.1 * nrm(ks[10], (L, ATT_HEAD_DIM)),
        'lambda_k1': 0.1 * nrm(ks[11], (L, ATT_HEAD_DIM)),
        'lambda_q2': 0.1 * nrm(ks[12], (L, ATT_HEAD_DIM)),
        'lambda_k2': 0.1 * nrm(ks[13], (L, ATT_HEAD_DIM)),
        'subln_g': 1.0 + 0.02 * nrm(ks[14], (L, ATT_V_DIM)),
        'pool_w': nrm(ks[15], (L, POOL_GROUPS, POOL_GROUP_DIM, POOL_GROUP_DIM)) * POOL_GROUP_DIM ** -0.5,
        'pool_scale': 1.0 + 0.1 * nrm(ks[16], (L, POOL_WIDTH)),
        'conv_w': nrm(ks[17], (L, CONV_K, CONV_WIDTH)) * CONV_K ** -0.5,
        'w_router': nrm(ks[18], (L, D, N_EXPERTS)) * D ** -0.5,
        'w_gate': nrm(ks[19], (L, N_EXPERTS, D, EXPERT_FF)) * D ** -0.5,
        'w_up': nrm(ks[20], (L, N_EXPERTS, D, EXPERT_FF)) * D ** -0.5,
        'w_down': nrm(ks[21], (L, N_EXPERTS, EXPERT_FF, D)) * EXPERT_FF ** -0.5,
        'final_g': 1.0 + 0.02 * nrm(ks[22], (D,)),
    }


def reference(x, c, ctx, c_ctx, w_ada, b_ada, norm1_g, norm2_g, w_in, w_out, lambda_q1, lambda_k1,
              lambda_q2, lambda_k2, subln_g, pool_w, pool_scale, conv_w, w_router, w_gate, w_up, w_down,
              final_g):
    B, n, D = x.shape
    rows = n // GRID_W
    row = jnp.repeat(jnp.arange(rows), GRID_W)
    col = jnp.tile(jnp.arange(GRID_W), rows)
    cos, sin = axial_rope_tables(row, col, x.dtype)
    sc = jax.nn.silu(c)
    scc = jax.nn.silu(c_ctx)
    xc = ctx
    for l in range(DEPTH):
        last = l == DEPTH - 1
        mod_x = (sc @ w_ada[l] + b_ada[l]).reshape(B, N_MOD, 1, D)
        mod_c = (scc @ w_ada[l] + b_ada[l]).reshape(N_MOD, D)
        lam_init = 0.8 - 0.6 * math.exp(-0.3 * l)
        lam = (jnp.exp(jnp.sum(lambda_q1[l] * lambda_k1[l]).astype(jnp.float32))
               - jnp.exp(jnp.sum(lambda_q2[l] * lambda_k2[l]).astype(jnp.float32)) + lam_init)
        hx = modulate(rmsnorm(x, norm1_g[l]), mod_x[:, 0], mod_x[:, 1])
        hc = modulate(rmsnorm(xc, norm1_g[l]), mod_c[0], mod_c[1])
        qx, kx, vx, px, bx, cx, ix = split_groups(hx @ w_in[l])
        qc, kc, vc, pc, bc, ccg, ic = split_groups(hc @ w_in[l])
        qx = apply_rope(qx, cos, sin)
        kx = apply_rope(kx, cos, sin)
        k_all = jnp.concatenate([kc, kx], axis=1)
        v_all = jnp.concatenate([vc, vx], axis=1)
        att_x = diff_attn_blocked(qx, k_all, v_all, lam)
        mix_x = mixer_output(att_x, px, bx, cx, ix, subln_g[l], lam_init, pool_w[l], pool_scale[l],
                             conv_w[l], w_out[l])
        x = x + mod_x[:, 2] * mix_x
        h2 = modulate(rmsnorm(x, norm2_g[l]), mod_x[:, 3], mod_x[:, 4])
        x = x + mod_x[:, 5] * expert_choice_ffn(h2, w_router[l], w_gate[l], w_up[l], w_down[l])
        if not last:
            att_c = diff_attn_core(qc, kc, vc, lam)
            mix_c = mixer_output(att_c, pc, bc, ccg, ic, subln_g[l], lam_init, pool_w[l], pool_scale[l],
                                 conv_w[l], w_out[l])
            xc = xc + mod_c[2] * mix_c
            h2c = modulate(rmsnorm(xc, norm2_g[l]), mod_c[3], mod_c[4])
            xc = xc + mod_c[5] * expert_choice_ffn(h2c, w_router[l], w_gate[l], w_up[l], w_down[l])
    return rmsnorm(x, final_g)
```

```python
import contextlib
import math
import numpy as np
import ml_dtypes
import concourse.bass as bass
import concourse.mybir as mybir
from concourse.bass_utils import run_bass_kernel_spmd

F32 = mybir.dt.float32
BF16 = mybir.dt.bfloat16
AF = mybir.ActivationFunctionType
ALU = mybir.AluOpType
AX = mybir.AxisListType
NPBF = ml_dtypes.bfloat16
KD = 8

D = 2048
SEQ = 8192
CTX = 256
DEPTH = 4
NE = 16
FF = 1024
EPS = 1e-6
TOK = 2048
HALO = 8
NL = TOK + 2 * HALO
NCX = CTX + 2 * HALO
NT = NL + NCX
LAT0 = HALO
CTX0 = NL + HALO
NO = TOK + CTX


class Prog:
    ENG = ['sp', 'act', 'dve', 'pool', 'pe']

    def __init__(self):
        self.nc = bass.Bass("TRN2", target_bir_lowering=False)
        self.lists = {e: [] for e in self.ENG}
        self.seq = {e: 0 for e in self.ENG}
        self.lastw = {}
        self.readers = {}
        self.seen = {e: {} for e in self.ENG}
        self.dcount = {'sp': 0, 'pool': 0, 'act': 0}
        self.stack = contextlib.ExitStack()
        self.semvals = {}

    def dram(self, name, shape, dt, kind="ExternalInput"):
        return self.nc.dram_tensor(name, list(shape), dt, kind=kind).ap()

    def sb(self, name, shape, dt=F32):
        return self.stack.enter_context(self.nc.sbuf_tensor(name, list(shape), dt))

    def ps(self, name, shape, dt=F32):
        return self.stack.enter_context(self.nc.psum_tensor(name, list(shape), dt))

    def _emit(self, E, fn, reads, writes, ev, inc, extra=()):
        waits = {}

        def need(e2):
            if e2 is None:
                return
            eng, s = e2
            if eng == 'pe' and E == 'pe':
                return
            if self.seen[E].get(eng, 0) >= s:
                return
            waits[eng] = max(waits.get(eng, 0), s)
        for r in reads:
            need(self.lastw.get(r))
        for w in writes:
            need(self.lastw.get(w))
            for eng, s in self.readers.get(w, {}).items():
                need((eng, s))
        for e2 in extra:
            need(e2)
        for eng, s in waits.items():
            self.seen[E][eng] = s
        for r in reads:
            d = self.readers.setdefault(r, {})
            d[ev[0]] = max(d.get(ev[0], 0), ev[1])
        for w in writes:
            self.lastw[w] = ev
            self.readers[w] = {}
        self.semvals[ev[0]] = max(self.semvals.get(ev[0], 0), ev[1])
        self.lists[E].append((list(waits.items()), fn, inc))

    def op(self, E, fn, reads=(), writes=(), inc=True):
        s = self.seq[E] + 1
        if inc:
            self.seq[E] = s
        self._emit(E, fn, reads, writes, (E, s), (E, 1) if inc else None)

    def dma(self, out, in_, reads=(), writes=(), Q='sp'):
        i = self.dcount[Q]
        self.dcount[Q] += 1
        name = 'd%s%d' % (Q, i % KD)
        val = 16 * (i // KD + 1)
        extra = [(name, val - 16)] if val > 16 else []
        self._emit(Q, lambda e: e.dma_start(out=out, in_=in_), reads, writes, (name, val), (name, 16), extra)

    def coll(self, kind, in_, out, reads=(), writes=(), groups=None):
        Q = 'pool'
        i = self.dcount[Q]
        self.dcount[Q] += 1
        name = 'd%s%d' % (Q, i % KD)
        val = 16 * (i // KD + 1)
        extra = [(name, val - 16)] if val > 16 else []
        rg = groups if groups is not None else [list(range(8))]
        self._emit(Q, lambda e: e.collective_compute(kind, ALU.bypass, replica_groups=rg, ins=[in_], outs=[out]),
                   reads, writes, (name, val), (name, 16), extra)

    def barrier(self):
        evs = list(self.semvals.items())
        for E in self.ENG:
            waits = []
            for eng, s in evs:
                if self.seen[E].get(eng, 0) < s:
                    waits.append((eng, s))
                    self.seen[E][eng] = s
            if waits:
                self.lists[E].append((waits, None, None))

    def finish(self):
        nc = self.nc
        self.barrier()
        names = sorted(self.semvals.keys())
        sems = {n: self.stack.enter_context(nc.semaphore(n)) for n in names}
        lists = self.lists
        with nc.Block() as block:
            def mk(E):
                def body(eng):
                    for waits, fn, inc in lists[E]:
                        for (s, v) in waits:
                            eng.wait_ge(sems[s], v)
                        if fn is not None:
                            ins = fn(eng)
                            if inc is not None:
                                ins.then_inc(sems[inc[0]], inc[1])
                return body
            block.sync(mk('sp'))
            block.scalar(mk('act'))
            block.vector(mk('dve'))
            block.gpsimd(mk('pool'))
            block.tensor(mk('pe'))
        self.stack.close()
        return nc


def build_A():
    p = Prog()
    xT = p.dram('xT', [D, NT], F32)
    mod = p.dram('mod', [128, 2, 6, 16], F32)
    g1 = p.dram('g1', [128, 16], F32)
    w_in = p.dram('w_in', [D, 5120], F32)
    cosT = p.dram('cosT', [128, TOK], F32)
    sinT = p.dram('sinT', [128, TOK], F32)
    perm = p.dram('perm', [128, 128], F32)
    invc = p.dram('invc', [4, NT], F32)
    valid = p.dram('valid', [128, 2], F32)
    pool_w = p.dram('pool_w', [4, 128, 128], F32)
    pool_s = p.dram('pool_s', [128, 4], F32)
    conv_w = p.dram('conv_w', [128, 4, 3], F32)
    qkvT = p.dram('qkvT', [3072, NO], BF16, kind="ExternalOutput")
    mixT = p.dram('mixT', [1024, NO], BF16, kind="ExternalOutput")

    hT = p.sb('hT', [128, 16, NT], BF16)
    scr = p.sb('scr', [128, 8192], F32)
    tmpA = p.sb('tmpA', [128, NT], F32)
    tmpB = p.sb('tmpB', [128, NT], F32)
    fb = p.sb('fb', [128, NT], BF16)
    cs = p.sb('cs', [128, 2, 512], F32)
    invb = p.sb('invb', [128, NT], F32)
    ws = p.sb('ws', [128, 16, 128], F32)
    wb = [p.sb('wb%d' % i, [128, 16, 128], BF16) for i in range(2)]
    ob = [p.sb('ob%d' % i, [128, NO], BF16) for i in range(2)]
    modsb = p.sb('modsb', [128, 2, 6, 16], F32)
    g1sb = p.sb('g1sb', [128, 16], F32)
    Asb = p.sb('Asb', [128, 2, 16], F32)
    ones = p.sb('ones', [128, 128], F32)
    permsb = p.sb('permsb', [128, 128], F32)
    validsb = p.sb('validsb', [128, 2], F32)
    pws = p.sb('pws', [128, 4, 128], F32)
    pwb = p.sb('pwb', [128, 4, 128], BF16)
    pssb = p.sb('pssb', [128, 4], F32)
    cwsb = p.sb('cwsb', [128, 4, 3], F32)
    sq = [p.sb('sq%d' % i, [128, 256], F32) for i in range(2)]
    rstd = p.sb('rstd', [128, 256], F32)
    t1 = [p.sb('t1_%d' % i, [128, 256], F32) for i in range(2)]
    pa = [p.ps('pa%d' % i, [128, 512]) for i in range(2)]
    pr = p.ps('pr', [128, 512])
    pp = p.ps('pp', [128, 512])
    pss = p.ps('pss', [128, 512])

    p.dma(modsb[:], mod, writes=['mod'])
    p.dma(g1sb[:], g1, writes=['g1'])
    p.dma(permsb[:], perm, writes=['perm'])
    p.dma(validsb[:], valid, writes=['valid'])
    p.dma(pws[:], pool_w.rearrange("g c d -> c g d"), writes=['pws'])
    p.dma(pssb[:], pool_s, writes=['pssb'])
    p.dma(cwsb[:], conv_w, writes=['cwsb'])
    p.op('dve', lambda e: e.memset(ones[:], 1.0), writes=['ones'])
    p.op('dve', lambda e: e.memset(tmpA[:], 0.0), writes=['tmpA'])
    p.op('dve', lambda e: e.memset(tmpB[:], 0.0), writes=['tmpB'])
    p.op('pool', lambda e: e.tensor_copy(out=pwb[:], in_=pws[:]), reads=['pws'], writes=['pwb'])
    for s in range(2):
        p.op('dve', lambda e, s=s: e.scalar_tensor_tensor(out=Asb[:, s, :], in0=modsb[:, s, 1, :], scalar=1.0,
                                                        in1=g1sb[:], op0=ALU.add, op1=ALU.mult),
             reads=['mod', 'g1'], writes=['A%d' % s])

    xTv = xT.rearrange("(k p) n -> p k n", p=128)
    ch1 = []
    c = 0
    while c < NL:
        n = min(256, NL - c)
        ch1.append((c, c + n, 0))
        c += n
    c = NL
    while c < NT:
        n = min(256, NT - c)
        ch1.append((c, c + n, 1))
        c += n
    for ci, (c0, c1, src) in enumerate(ch1):
        n = c1 - c0
        xi = ci % 2
        xs = scr[:, xi * 4096: xi * 4096 + 16 * 256].rearrange("p (k n) -> p k n", k=16)
        p.dma(xs[:, :, 0:n], xTv[:, :, c0:c1], writes=['xs%d' % xi])
        for k in range(16):
            si = k % 2
            p.op('act', lambda e, k=k, si=si, xs=xs, n=n: e.activation(out=sq[si][:, 0:n], in_=xs[:, k, 0:n], func=AF.Square),
                 reads=['xs%d' % xi], writes=['sq%d' % si])
            p.op('pe', lambda e, k=k, si=si, n=n: e.matmul(pss[:, 0:n], lhsT=ones[:], rhs=sq[si][:, 0:n],
                                                         start=(k == 0), stop=(k == 15)),
                 reads=['sq%d' % si, 'ones'], writes=['pss'])
        p.op('dve', lambda e, n=n: e.tensor_scalar(out=rstd[:, 0:n], in0=pss[:, 0:n], scalar1=1.0 / D, scalar2=EPS,
                                                   op0=ALU.mult, op1=ALU.add), reads=['pss'], writes=['rstd'])
        p.op('act', lambda e, n=n: e.activation(out=rstd[:, 0:n], in_=rstd[:, 0:n], func=AF.Sqrt),
             reads=['rstd'], writes=['rstd'])
        p.op('dve', lambda e, n=n: e.reciprocal(out=rstd[:, 0:n], in_=rstd[:, 0:n]), reads=['rstd'], writes=['rstd'])
        for k in range(16):
            ti = k % 2
            p.op('dve', lambda e, k=k, ti=ti, xs=xs, n=n, src=src: e.scalar_tensor_tensor(
                out=t1[ti][:, 0:n], in0=xs[:, k, 0:n], scalar=Asb[:, src, k:k + 1], in1=rstd[:, 0:n],
                op0=ALU.mult, op1=ALU.mult), reads=['xs%d' % xi, 'rstd', 'A%d' % src], writes=['t1_%d' % ti])
            p.op('act', lambda e, k=k, ti=ti, n=n, src=src, c0=c0, c1=c1: e.activation(
                out=hT[:, k, c0:c1], in_=t1[ti][:, 0:n], func=AF.Identity, bias=modsb[:, src, 0, k:k + 1], scale=1.0),
                reads=['t1_%d' % ti, 'mod'], writes=['hT'])
    p.barrier()

    rows = [scr[:, i * NT:(i + 1) * NT] for i in range(3)]
    w_v = w_in.rearrange("(k p) c -> p k c", p=128)
    ch2 = [(0, 512), (512, 1024), (1024, 1536), (1536, 2048), (2048, NT)]
    own = [(LAT0 + 512 * i, LAT0 + 512 * (i + 1), 512 * i) for i in range(4)] + [(CTX0, CTX0 + CTX, TOK)]
    state = {'tile': 0, 'pa': 0, 'ob': 0}

    def proj(col, r):
        ti = state['tile']
        state['tile'] += 1
        bi = ti % 2
        p.dma(ws[:], w_v[:, :, col:col + 128], writes=['ws'])
        p.op('pool', lambda e: e.tensor_copy(out=wb[bi][:], in_=ws[:]), reads=['ws'], writes=['wb%d' % bi])
        for (c0, c1) in ch2:
            pi = state['pa'] % 2
            state['pa'] += 1
            n = c1 - c0
            for k in range(16):
                p.op('pe', lambda e, k=k, pi=pi, n=n, c0=c0, c1=c1: e.matmul(
                    pa[pi][:, 0:n], lhsT=wb[bi][:, k, :], rhs=hT[:, k, c0:c1], start=(k == 0), stop=(k == 15)),
                    reads=['wb%d' % bi, 'hT'], writes=['pa%d' % pi], inc=(k == 15))
            p.op('act', lambda e, pi=pi, n=n, c0=c0, c1=c1: e.copy(out=rows[r][:, c0:c1], in_=pa[pi][:, 0:n]),
                 reads=['pa%d' % pi], writes=['row%d' % r])

    def zero_halos(buf, key):
        p.op('dve', lambda e: e.tensor_scalar(out=buf[:, 0:HALO], in0=buf[:, 0:HALO], scalar1=validsb[:, 0:1],
                                              scalar2=None, op0=ALU.mult), reads=[key, 'valid'], writes=[key])
        p.op('dve', lambda e: e.tensor_scalar(out=buf[:, NL - HALO:NL], in0=buf[:, NL - HALO:NL],
                                              scalar1=validsb[:, 1:2], scalar2=None, op0=ALU.mult),
             reads=[key, 'valid'], writes=[key])
        p.op('dve', lambda e: e.memset(buf[:, NL:NL + HALO], 0.0), reads=[key], writes=[key])
        p.op('dve', lambda e: e.memset(buf[:, NT - HALO:NT], 0.0), reads=[key], writes=[key])

    def next_ob():
        oi = state['ob'] % 2
        state['ob'] += 1
        return oi

    for j in range(24):
        r = j % 3
        proj(j * 128, r)
        oi = next_ob()
        R = rows[r]
        rk = 'row%d' % r
        ok = 'ob%d' % oi
        if j < 16:
            for c in range(4):
                a0, a1, o0 = own[c]
                p.dma(cs[:, 0, :], cosT[:, o0:o0 + 512], writes=['cs'])
                p.dma(cs[:, 1, :], sinT[:, o0:o0 + 512], writes=['cs'])
                p.op('pe', lambda e, a0=a0, a1=a1, R=R: e.matmul(pr[:], lhsT=permsb[:], rhs=R[:, a0:a1], start=True, stop=True),
                     reads=[rk, 'perm'], writes=['pr'])
                p.op('dve', lambda e, a0=a0, a1=a1, R=R: e.tensor_tensor(out=tmpA[:, 0:512], in0=R[:, a0:a1], in1=cs[:, 0, :], op=ALU.mult),
                     reads=[rk, 'cs'], writes=['tmpA'])
                p.op('dve', lambda e: e.tensor_tensor(out=tmpB[:, 0:512], in0=pr[:], in1=cs[:, 1, :], op=ALU.mult),
                     reads=['pr', 'cs'], writes=['tmpB'])
                p.op('dve', lambda e, o0=o0, oi=oi: e.tensor_tensor(out=ob[oi][:, o0:o0 + 512], in0=tmpA[:, 0:512], in1=tmpB[:, 0:512], op=ALU.add),
                     reads=['tmpA', 'tmpB'], writes=[ok])
        else:
            p.op('act', lambda e, R=R, oi=oi: e.copy(out=ob[oi][:, 0:TOK], in_=R[:, LAT0:LAT0 + TOK]), reads=[rk], writes=[ok])
        p.op('act', lambda e, R=R, oi=oi: e.copy(out=ob[oi][:, TOK:NO], in_=R[:, CTX0:CTX0 + CTX]), reads=[rk], writes=[ok])
        p.dma(qkvT[j * 128:(j + 1) * 128, :], ob[oi][:], reads=[ok], Q='pool')

    for g in range(4):
        r = g % 3
        proj(3072 + g * 128, r)
        R = rows[r]
        rk = 'row%d' % r
        zero_halos(R, rk)
        p.dma(invb[:], invc[g, :].partition_broadcast(128), writes=['invb'])
        p.op('dve', lambda e, R=R: e.tensor_tensor(out=tmpA[:, 1:NT], in0=R[:, 0:NT - 1], in1=R[:, 1:NT], op=ALU.add),
             reads=[rk], writes=['tmpA'])
        S, O, sk, okk = tmpA, tmpB, 'tmpA', 'tmpB'
        if g >= 1:
            p.op('dve', lambda e: e.tensor_tensor(out=tmpB[:, 2:NT - 1], in0=tmpA[:, 1:NT - 2], in1=tmpA[:, 3:NT], op=ALU.add),
                 reads=['tmpA'], writes=['tmpB'])
            S, O, sk, okk = tmpB, tmpA, 'tmpB', 'tmpA'
        if g >= 2:
            p.op('dve', lambda e: e.tensor_tensor(out=tmpA[:, 4:NT - 3], in0=tmpB[:, 2:NT - 5], in1=tmpB[:, 6:NT - 1], op=ALU.add),
                 reads=['tmpB'], writes=['tmpA'])
            S, O, sk, okk = tmpA, tmpB, 'tmpA', 'tmpB'
        if g >= 3:
            p.op('dve', lambda e: e.tensor_tensor(out=tmpB[:, 8:NT - 7], in0=tmpA[:, 4:NT - 11], in1=tmpA[:, 12:NT - 3], op=ALU.add),
                 reads=['tmpA'], writes=['tmpB'])
            S, O, sk, okk = tmpB, tmpA, 'tmpB', 'tmpA'
        p.op('dve', lambda e, S=S, O=O: e.tensor_tensor(out=O[:, 8:NT - 8], in0=S[:, 8:NT - 8], in1=invb[:, 8:NT - 8], op=ALU.mult),
             reads=[sk, 'invb'], writes=[okk])
        p.op('dve', lambda e, O=O, R=R: e.tensor_tensor(out=fb[:, 8:NT - 8], in0=O[:, 8:NT - 8], in1=R[:, 8:NT - 8], op=ALU.subtract),
             reads=[okk, rk], writes=['fb'])
        oi = next_ob()
        ok = 'ob%d' % oi
        for (a0, a1, o0) in own:
            n = a1 - a0
            p.op('pe', lambda e, a0=a0, a1=a1, n=n, g=g: e.matmul(pp[:, 0:n], lhsT=pwb[:, g, :], rhs=fb[:, a0:a1], start=True, stop=True),
                 reads=['fb', 'pwb'], writes=['pp'])
            p.op('dve', lambda e, n=n, o0=o0, oi=oi, g=g: e.tensor_scalar(out=ob[oi][:, o0:o0 + n], in0=pp[:, 0:n], scalar1=pssb[:, g:g + 1],
                                                                   scalar2=None, op0=ALU.mult), reads=['pp', 'pssb'], writes=[ok])
        p.dma(mixT[g * 128:(g + 1) * 128, :], ob[oi][:], reads=[ok], Q='pool')

    for i in range(4):
        proj(3584 + i * 128, 0)
        proj(4096 + i * 128, 1)
        proj(4608 + i * 128, 2)
        p.op('dve', lambda e: e.tensor_tensor(out=tmpA[:], in0=rows[1][:], in1=rows[2][:], op=ALU.mult),
             reads=['row1', 'row2'], writes=['tmpA'])
        zero_halos(tmpA, 'tmpA')
        p.op('dve', lambda e, i=i: e.tensor_scalar(out=tmpB[:, 1:NT - 1], in0=tmpA[:, 0:NT - 2], scalar1=cwsb[:, i, 0:1], scalar2=None, op0=ALU.mult),
             reads=['tmpA', 'cwsb'], writes=['tmpB'])
        p.op('dve', lambda e, i=i: e.scalar_tensor_tensor(out=tmpB[:, 1:NT - 1], in0=tmpA[:, 1:NT - 1], scalar=cwsb[:, i, 1:2], in1=tmpB[:, 1:NT - 1],
                                                         op0=ALU.mult, op1=ALU.add), reads=['tmpA', 'tmpB', 'cwsb'], writes=['tmpB'])
        p.op('dve', lambda e, i=i: e.scalar_tensor_tensor(out=tmpB[:, 1:NT - 1], in0=tmpA[:, 2:NT], scalar=cwsb[:, i, 2:3], in1=tmpB[:, 1:NT - 1],
                                                         op0=ALU.mult, op1=ALU.add), reads=['tmpA', 'tmpB', 'cwsb'], writes=['tmpB'])
        oi = next_ob()
        ok = 'ob%d' % oi
        p.op('dve', lambda e, oi=oi: e.tensor_tensor(out=ob[oi][:, 0:TOK], in0=tmpB[:, LAT0:LAT0 + TOK], in1=rows[0][:, LAT0:LAT0 + TOK], op=ALU.mult),
             reads=['tmpB', 'row0'], writes=[ok])
        p.op('dve', lambda e, oi=oi: e.tensor_tensor(out=ob[oi][:, TOK:NO], in0=tmpB[:, CTX0:CTX0 + CTX], in1=rows[0][:, CTX0:CTX0 + CTX], op=ALU.mult),
             reads=['tmpB', 'row0'], writes=[ok])
        p.dma(mixT[512 + i * 128:512 + (i + 1) * 128, :], ob[oi][:], reads=[ok], Q='pool')
    return p.finish()


def _pk(v):
    v = np.asarray(v, np.float32)
    lead = v.shape[:-1]
    return np.ascontiguousarray(np.moveaxis(v.reshape(lead + (16, 128)), -1, 0))


def _rope_tables(s):
    t = s * TOK + np.arange(TOK)
    row = (t // 64).astype(np.float32)
    col = (t % 64).astype(np.float32)
    inv = (1.0 / (10000.0 ** (np.arange(0, 32, 2, dtype=np.float32) / 32.0))).astype(np.float32)
    ang = np.concatenate([row[:, None] * inv, col[:, None] * inv], axis=-1).astype(np.float32)
    cos = np.cos(ang).astype(np.float32).T
    sin = np.sin(ang).astype(np.float32).T
    cosT = np.tile(cos, (4, 1))
    sinT = np.concatenate([-sin, sin, -sin, sin], axis=0)
    return np.ascontiguousarray(cosT), np.ascontiguousarray(sinT)


def _invcnt(s):
    out = np.zeros((4, NT), np.float32)
    for g, w in enumerate((2, 4, 8, 16)):
        t = s * TOK + np.arange(TOK)
        lo = np.clip(t - w // 2, 0, SEQ)
        hi = np.clip(t + w - w // 2, 0, SEQ)
        out[g, LAT0:LAT0 + TOK] = 1.0 / (hi - lo).astype(np.float32)
        t = np.arange(CTX)
        lo = np.clip(t - w // 2, 0, CTX)
        hi = np.clip(t + w - w // 2, 0, CTX)
        out[g, CTX0:CTX0 + CTX] = 1.0 / (hi - lo).astype(np.float32)
    return out


def _perm():
    P = np.zeros((128, 128), np.float32)
    for i in range(128):
        P[i, i ^ 32] = 1.0
    return P


def _halo_xT(xb, s, xcb):
    out = np.zeros((D, NT), np.float32)
    lo = s * TOK - HALO
    hi = (s + 1) * TOK + HALO
    a, b = max(lo, 0), min(hi, SEQ)
    out[:, a - lo:a - lo + (b - a)] = xb[a:b].T
    out[:, CTX0:CTX0 + CTX] = xcb.T
    return out


def inputs_A(l, core, x, xc, mods, W):
    b, s = core // 4, core % 4
    cosT, sinT = _rope_tables(s)
    mod = np.stack([_pk(mods[l, b]), _pk(mods[l, 2])], axis=1)
    return {
        'xT': _halo_xT(x[b], s, xc[b]),
        'mod': np.ascontiguousarray(mod),
        'g1': _pk(W['norm1_g'][l]),
        'w_in': W['w_in'][l],
        'cosT': cosT, 'sinT': sinT, 'perm': _perm(), 'invc': _invcnt(s),
        'valid': np.tile(np.array([[1.0 if s > 0 else 0.0, 1.0 if s < 3 else 0.0]], np.float32), (128, 1)),
        'pool_w': W['pool_w'][l],
        'pool_s': np.ascontiguousarray(W['pool_scale'][l].reshape(4, 128).T),
        'conv_w': np.ascontiguousarray(W['conv_w'][l].reshape(3, 4, 128).transpose(2, 1, 0)),
    }


NK = SEQ + CTX
KT = NK // 128
CHS = [(0, 512, 0), (512, 1024, 0), (1024, 1536, 0), (1536, 2048, 0), (2048, NO, 1)]


def build_B():
    p = Prog()
    qT = p.dram('qT', [1024, NO], BF16)
    kT = p.dram('kT', [1024, NK], BF16)
    vh = p.dram('vh', [8, 128, NK], BF16)
    mixT = p.dram('mixT', [1024, NO], BF16)
    xT = p.dram('xT', [D, NO], F32)
    w_out = p.dram('w_out', [D, D], F32)
    mod = p.dram('mod', [128, 2, 6, 16], F32)
    g2 = p.dram('g2', [128, 16], F32)
    sc = p.dram('sc', [128, 3], F32)
    w_r = p.dram('w_r', [128, 16, 16], F32)
    xmidT = p.dram('xmidT', [D, NO], F32, kind="ExternalOutput")
    h2T = p.dram('h2T', [D, NO], BF16, kind="ExternalOutput")
    logT = p.dram('logT', [16, NO], F32, kind="ExternalOutput")

    kvf = p.sb('kvf', [128, NK], F32)
    kvb = kvf[:].bitcast(BF16)
    kTh = kvb[:, 0:NK]
    vflat = kvb[:, NK:2 * NK]
    vhs = vflat.rearrange("p (t e) -> p t e", e=128)
    qh = p.sb('qh', [128, NO], BF16)
    ymix = p.sb('ymix', [128, 16, NO], BF16)
    pT = [p.sb('pT%d' % i, [128, 512], BF16) for i in range(2)]
    am = [p.sb('am%d' % i, [128, 512], F32) for i in range(2)]
    rl = p.sb('rl', [128, 512], F32)
    att = p.sb('att', [128, 512], F32)
    sqa = p.sb('sqa', [128, 512], F32)
    rs = p.sb('rs', [128, 512], F32)
    onesb = p.sb('onesb', [128, 128], BF16)
    onesf = p.sb('onesf', [128, 128], F32)
    modsb = p.sb('modsb', [128, 2, 6, 16], F32)
    g2sb = p.sb('g2sb', [128, 16], F32)
    A2 = p.sb('A2', [128, 2, 16], F32)
    scsb = p.sb('scsb', [128, 3], F32)
    sg = p.sb('sg', [128, 1], F32)
    neglam = p.sb('neglam', [128, 1], F32)
    wrsb = p.sb('wrsb', [128, 16, 16], F32)
    ws = p.sb('ws', [128, 16, 128], F32)
    wb = [p.sb('wb%d' % i, [128, 16, 128], BF16) for i in range(2)]
    rstd = p.sb('rstd', [128, NO], F32)
    h2f = [p.sb('h2f%d' % i, [128, NO], F32) for i in range(2)]
    h2b = [p.sb('h2b%d' % i, [128, NO], BF16) for i in range(2)]
    lgs = p.sb('lgs', [16, NO], F32)
    psS = [p.ps('psS%d' % i, [128, 512]) for i in range(2)]
    psO = [p.ps('psO%d' % i, [128, 512]) for i in range(2)]
    psL = [p.ps('psL%d' % i, [128, 512]) for i in range(2)]
    psN = p.ps('psN', [128, 512])

    p.dma(modsb[:], mod, writes=['mod'])
    p.dma(g2sb[:], g2, writes=['g2'])
    p.dma(scsb[:], sc, writes=['sc'])
    p.dma(wrsb[:], w_r, writes=['wr'])
    for i in range(8):
        p.dma(ymix[:, 8 + i, :], mixT[i * 128:(i + 1) * 128, :], writes=['ymixp'])
    p.op('dve', lambda e: e.memset(onesb[:], 1.0), writes=['onesb'])
    p.op('dve', lambda e: e.memset(onesf[:], 1.0), writes=['onesf'])
    p.op('dve', lambda e: e.tensor_tensor(out=sg[:], in0=scsb[:, 1:2], in1=scsb[:, 2:3], op=ALU.mult), reads=['sc'], writes=['sg'])
    p.op('dve', lambda e: e.tensor_scalar(out=neglam[:], in0=scsb[:, 0:1], scalar1=-1.0, scalar2=None, op0=ALU.mult),
         reads=['sc'], writes=['neglam'])
    for s in range(2):
        p.op('dve', lambda e, s=s: e.scalar_tensor_tensor(out=A2[:, s, :], in0=modsb[:, s, 4, :], scalar=1.0,
                                                        in1=g2sb[:], op0=ALU.add, op1=ALU.mult),
             reads=['mod', 'g2'], writes=['A2'])

    cnt = 0
    for h in range(8):
        p.dma(kTh, kT[h * 128:(h + 1) * 128, :], writes=['kTh'])
        p.dma(vflat, vh[h], writes=['vhs'])
        p.dma(qh[:], qT[h * 128:(h + 1) * 128, :], writes=['qh'])
        for (q0, q1, src) in CHS:
            n = q1 - q0
            nkt = KT if src == 0 else CTX // 128
            for m in range(2):
                for kt in range(nkt):
                    si = cnt % 2
                    cnt += 1
                    p.op('pe', lambda e, si=si, m=m, kt=kt, n=n, q0=q0, q1=q1: e.matmul(
                        psS[si][:, 0:n], lhsT=kTh[m * 64:(m + 1) * 64, kt * 128:(kt + 1) * 128],
                        rhs=qh[m * 64:(m + 1) * 64, q0:q1], start=True, stop=True),
                        reads=['kTh', 'qh'], writes=['psS%d' % si])
                    p.op('act', lambda e, si=si, n=n: e.activation(out=pT[si][:, 0:n], in_=psS[si][:, 0:n], func=AF.Exp, scale=0.125),
                         reads=['psS%d' % si], writes=['pT%d' % si])
                    p.op('pe', lambda e, si=si, m=m, kt=kt, n=n, nkt=nkt: e.matmul(
                        psO[m][:, 0:n], lhsT=vhs[:, kt, :], rhs=pT[si][:, 0:n], start=(kt == 0), stop=(kt == nkt - 1)),
                        reads=['vhs', 'pT%d' % si], writes=['psO%d' % m], inc=False)
                    p.op('pe', lambda e, si=si, m=m, kt=kt, n=n, nkt=nkt: e.matmul(
                        psL[m][:, 0:n], lhsT=onesb[:], rhs=pT[si][:, 0:n], start=(kt == 0), stop=(kt == nkt - 1)),
                        reads=['onesb', 'pT%d' % si], writes=['psL%d' % m])
                p.op('dve', lambda e, m=m, n=n: e.reciprocal(out=rl[:, 0:n], in_=psL[m][:, 0:n]), reads=['psL%d' % m], writes=['rl'])
                p.op('dve', lambda e, m=m, n=n: e.tensor_tensor(out=am[m][:, 0:n], in0=psO[m][:, 0:n], in1=rl[:, 0:n], op=ALU.mult),
                     reads=['psO%d' % m, 'rl'], writes=['am%d' % m])
            p.op('dve', lambda e, n=n: e.scalar_tensor_tensor(out=att[:, 0:n], in0=am[1][:, 0:n], scalar=neglam[:, 0:1], in1=am[0][:, 0:n],
                                                           op0=ALU.mult, op1=ALU.add), reads=['am0', 'am1', 'neglam'], writes=['att'])
            p.op('act', lambda e, n=n: e.activation(out=sqa[:, 0:n], in_=att[:, 0:n], func=AF.Square), reads=['att'], writes=['sqa'])
            p.op('pe', lambda e, n=n: e.matmul(psN[:, 0:n], lhsT=onesf[:], rhs=sqa[:, 0:n], start=True, stop=True),
                 reads=['sqa', 'onesf'], writes=['psN'])
            p.op('dve', lambda e, n=n: e.tensor_scalar(out=rs[:, 0:n], in0=psN[:, 0:n], scalar1=1.0 / 128, scalar2=EPS, op0=ALU.mult, op1=ALU.add),
                 reads=['psN'], writes=['rs'])
            p.op('act', lambda e, n=n: e.activation(out=rs[:, 0:n], in_=rs[:, 0:n], func=AF.Sqrt), reads=['rs'], writes=['rs'])
            p.op('dve', lambda e, n=n: e.reciprocal(out=rs[:, 0:n], in_=rs[:, 0:n]), reads=['rs'], writes=['rs'])
            p.op('dve', lambda e, n=n, h=h, q0=q0, q1=q1: e.scalar_tensor_tensor(out=ymix[:, h, q0:q1], in0=att[:, 0:n], scalar=sg[:, 0:1], in1=rs[:, 0:n],
                                                                           op0=ALU.mult, op1=ALU.mult), reads=['att', 'rs', 'sg'], writes=['ymixa'])
    p.barrier()

    xr = [kvf[:, 0:NO], kvf[:, NO:2 * NO]]
    sqb = kvf[:, 2 * NO:3 * NO]
    pq = [psO[0], psO[1], psL[0], psL[1], psN]
    wo_v = w_out.rearrange("(m p) c -> p m c", p=128)
    pac = 0
    for dt in range(16):
        bi = dt % 2
        xi = dt % 2
        p.dma(ws[:], wo_v[:, :, dt * 128:(dt + 1) * 128], writes=['ws'])
        p.op('pool', lambda e, bi=bi: e.tensor_copy(out=wb[bi][:], in_=ws[:]), reads=['ws'], writes=['wb%d' % bi])
        p.dma(xr[xi], xT[dt * 128:(dt + 1) * 128, :], writes=['xr%d' % xi])
        for ci, (c0, c1, src) in enumerate(CHS):
            n = c1 - c0
            pi = pac % 2
            pac += 1
            for m in range(16):
                p.op('pe', lambda e, m=m, bi=bi, pi=pi, n=n, c0=c0, c1=c1: e.matmul(
                    psS[pi][:, 0:n], lhsT=wb[bi][:, m, :], rhs=ymix[:, m, c0:c1], start=(m == 0), stop=(m == 15)),
                    reads=['wb%d' % bi, 'ymixa', 'ymixp'], writes=['psS%d' % pi], inc=(m == 15))
            p.op('dve', lambda e, pi=pi, n=n, c0=c0, c1=c1, src=src, dt=dt, xi=xi: e.scalar_tensor_tensor(
                out=xr[xi][:, c0:c1], in0=psS[pi][:, 0:n], scalar=modsb[:, src, 2, dt:dt + 1], in1=xr[xi][:, c0:c1],
                op0=ALU.mult, op1=ALU.add), reads=['psS%d' % pi, 'xr%d' % xi, 'mod'], writes=['xr%d' % xi])
        p.op('act', lambda e, xi=xi: e.activation(out=sqb, in_=xr[xi], func=AF.Square), reads=['xr%d' % xi], writes=['sqb'])
        for ci, (c0, c1, src) in enumerate(CHS):
            n = c1 - c0
            p.op('pe', lambda e, ci=ci, n=n, c0=c0, c1=c1, dt=dt: e.matmul(pq[ci][:, 0:n], lhsT=onesf[:], rhs=sqb[:, c0:c1],
                                                                     start=(dt == 0), stop=(dt == 15)),
                 reads=['sqb', 'onesf'], writes=['pq%d' % ci])
        p.dma(xmidT[dt * 128:(dt + 1) * 128, :], xr[xi], reads=['xr%d' % xi], writes=['xmid%d' % dt], Q='pool')
    for ci, (c0, c1, src) in enumerate(CHS):
        n = c1 - c0
        p.op('dve', lambda e, ci=ci, n=n, c0=c0, c1=c1: e.tensor_scalar(out=rstd[:, c0:c1], in0=pq[ci][:, 0:n], scalar1=1.0 / D, scalar2=EPS,
                                                                  op0=ALU.mult, op1=ALU.add), reads=['pq%d' % ci], writes=['rstd'])
    p.op('act', lambda e: e.activation(out=rstd[:], in_=rstd[:], func=AF.Sqrt), reads=['rstd'], writes=['rstd'])
    p.op('dve', lambda e: e.reciprocal(out=rstd[:], in_=rstd[:]), reads=['rstd'], writes=['rstd'])
    for k in range(16):
        xi = k % 2
        hi = k % 2
        p.dma(xr[xi], xmidT[k * 128:(k + 1) * 128, :], reads=['xmid%d' % k], writes=['xr%d' % xi])
        for (c0, c1, src) in [(0, TOK, 0), (TOK, NO, 1)]:
            p.op('dve', lambda e, xi=xi, c0=c0, c1=c1, src=src, k=k: e.scalar_tensor_tensor(
                out=xr[xi][:, c0:c1], in0=xr[xi][:, c0:c1], scalar=A2[:, src, k:k + 1], in1=rstd[:, c0:c1],
                op0=ALU.mult, op1=ALU.mult), reads=['xr%d' % xi, 'rstd', 'A2'], writes=['xr%d' % xi])
            p.op('act', lambda e, xi=xi, hi=hi, c0=c0, c1=c1, src=src, k=k: e.activation(
                out=h2f[hi][:, c0:c1], in_=xr[xi][:, c0:c1], func=AF.Identity, bias=modsb[:, src, 3, k:k + 1], scale=1.0),
                reads=['xr%d' % xi, 'mod'], writes=['h2f%d' % hi])
        p.op('pool', lambda e, hi=hi: e.tensor_copy(out=h2b[hi][:], in_=h2f[hi][:]), reads=['h2f%d' % hi], writes=['h2b%d' % hi])
        p.dma(h2T[k * 128:(k + 1) * 128, :], h2b[hi][:], reads=['h2b%d' % hi], Q='pool')
        for ci, (c0, c1, src) in enumerate(CHS):
            n = c1 - c0
            p.op('pe', lambda e, ci=ci, n=n, c0=c0, c1=c1, k=k, hi=hi: e.matmul(pq[ci][0:16, 0:n], lhsT=wrsb[:, k, :], rhs=h2f[hi][:, c0:c1],
                                                                          start=(k == 0), stop=(k == 15)),
                 reads=['h2f%d' % hi, 'wr', 'rstd'], writes=['pq%d' % ci])
    for ci, (c0, c1, src) in enumerate(CHS):
        n = c1 - c0
        p.op('act', lambda e, ci=ci, n=n, c0=c0, c1=c1: e.copy(out=lgs[:, c0:c1], in_=pq[ci][0:16, 0:n]), reads=['pq%d' % ci], writes=['lgs'])
    p.dma(logT, lgs[:], reads=['lgs'], Q='pool')
    return p.finish()


NA = SEQ + CTX
GT = 768
NBIS = 34


def build_C():
    p = Prog()
    logA = p.dram('logA', [16, NA], F32)
    logO = p.dram('logO', [16, NO], F32)
    h2T = p.dram('h2T', [D, NO], BF16)
    xmidT = p.dram('xmidT', [D, NO], F32)
    mod = p.dram('mod', [128, 2, 6, 16], F32)
    fg = p.dram('fg', [128, 16], F32)
    sel = p.dram('sel', [16, 16, 128], F32)
    w_gate = p.dram('w_gate', [NE, D, FF], F32)
    w_up = p.dram('w_up', [NE, D, FF], F32)
    w_down = p.dram('w_down', [NE, FF, D], F32)
    xoutT = p.dram('xoutT', [D, NO], F32, kind="ExternalOutput")
    xnT = p.dram('xnT', [D, NO], F32, kind="ExternalOutput")

    accf = p.sb('accf', [128, 16 * GT], F32)
    acc = accf[:].rearrange("p (k n) -> p k n", k=16)
    cmp = accf[0:16, 0:SEQ]
    wdf = p.sb('wdf', [128, NA], F32)
    eA = wdf[0:16, :]
    wdb = wdf[:].bitcast(BF16)[:, 0:8 * D].rearrange("p (f d) -> p f d", f=8)
    h2g = p.sb('h2g', [128, 16, GT], BF16)
    s3 = p.sb('s3', [128, 8, GT], BF16)
    wgs = p.sb('wgs', [128, 16, 128], F32)
    wus = p.sb('wus', [128, 16, 128], F32)
    wds = p.sb('wds', [128, D], F32)
    wgb = [p.sb('wgb%d' % i, [128, 16, 128], BF16) for i in range(2)]
    wub = [p.sb('wub%d' % i, [128, 16, 128], BF16) for i in range(2)]
    gbc = [p.sb('gbc%d' % i, [128, 512], F32) for i in range(2)]
    sil = [p.sb('sil%d' % i, [128, 512], F32) for i in range(2)]
    eO = p.sb('eO', [16, NO], F32)
    gsel = p.sb('gsel', [16, NO], F32)
    rc = p.sb('rc', [16, 512], F32)
    ones16 = p.sb('ones16', [16, 16], F32)
    onesf = p.sb('onesf', [128, 128], F32)
    selsb = p.sb('selsb', [16, 16, 128], F32)
    modsb = p.sb('modsb', [128, 2, 6, 16], F32)
    fgsb = p.sb('fgsb', [128, 16], F32)
    xm = [p.sb('xm%d' % i, [128, GT], F32) for i in range(2)]
    rstd = p.sb('rstd', [128, GT], F32)
    bs = {nm: p.sb('bs_' + nm, [16, 2], F32) for nm in ['lo', 'hi', 'mid', 'cnt', 'pred', 'npred', 'a', 'b']}
    pA = [p.ps('pA%d' % i, [128, 512]) for i in range(2)]
    pU = [p.ps('pU%d' % i, [128, 512]) for i in range(2)]
    pD = [p.ps('pD%d' % i, [128, 512]) for i in range(2)]
    pG = p.ps('pG', [128, 512])

    p.dma(eA, logA, writes=['eA'])
    p.dma(eO[:], logO, writes=['eO'])
    p.dma(modsb[:], mod, writes=['mod'])
    p.dma(fgsb[:], fg, writes=['fg'])
    p.dma(selsb[:], sel, writes=['sel'])
    p.op('dve', lambda e: e.memset(ones16[:], 1.0), writes=['ones16'])
    p.op('dve', lambda e: e.memset(onesf[:], 1.0), writes=['onesf'])

    def softmax_cols(buf, key, ncols):
        p.op('act', lambda e: e.activation(out=buf, in_=buf, func=AF.Exp), reads=[key], writes=[key])
        c = 0
        while c < ncols:
            n = min(512, ncols - c)
            p.op('pe', lambda e, c=c, n=n: e.matmul(pG[0:16, 0:n], lhsT=ones16[:], rhs=buf[:, c:c + n], start=True, stop=True),
                 reads=[key, 'ones16'], writes=['pG'])
            p.op('dve', lambda e, n=n: e.reciprocal(out=rc[:, 0:n], in_=pG[0:16, 0:n]), reads=['pG'], writes=['rc'])
            p.op('dve', lambda e, c=c, n=n: e.tensor_tensor(out=buf[:, c:c + n], in0=buf[:, c:c + n], in1=rc[:, 0:n], op=ALU.mult),
                 reads=[key, 'rc'], writes=[key])
            c += n
    softmax_cols(eA, 'eA', NA)
    softmax_cols(eO[:], 'eO', NO)

    p.op('dve', lambda e: e.memset(bs['lo'][:], 0.0), writes=['lo'])
    p.op('dve', lambda e: e.memset(bs['hi'][:], 1.5), writes=['hi'])
    sets = [(0, 0, SEQ, 2 * SEQ // NE), (1, SEQ, NA, 2 * CTX // NE)]
    for it in range(NBIS):
        p.op('dve', lambda e: e.tensor_tensor(out=bs['mid'][:], in0=bs['lo'][:], in1=bs['hi'][:], op=ALU.add), reads=['lo', 'hi'], writes=['mid'])
        p.op('dve', lambda e: e.tensor_scalar(out=bs['mid'][:], in0=bs['mid'][:], scalar1=0.5, scalar2=None, op0=ALU.mult), reads=['mid'], writes=['mid'])
        for (j, c0, c1, kk) in sets:
            n = c1 - c0
            p.op('dve', lambda e, j=j, c0=c0, c1=c1, n=n: e.tensor_scalar(out=cmp[:, 0:n], in0=eA[:, c0:c1], scalar1=bs['mid'][:, j:j + 1], scalar2=None,
                                                                    op0=ALU.is_ge), reads=['eA', 'mid'], writes=['cmp'])
            p.op('dve', lambda e, j=j, n=n: e.reduce_sum(out=bs['cnt'][:, j:j + 1], in_=cmp[:, 0:n], axis=AX.X), reads=['cmp'], writes=['cnt'])
            p.op('dve', lambda e, j=j, kk=kk: e.tensor_scalar(out=bs['pred'][:, j:j + 1], in0=bs['cnt'][:, j:j + 1], scalar1=kk - 0.5, scalar2=None,
                                                            op0=ALU.is_ge), reads=['cnt'], writes=['pred'])
        p.op('dve', lambda e: e.tensor_scalar(out=bs['npred'][:], in0=bs['pred'][:], scalar1=-1.0, scalar2=1.0, op0=ALU.mult, op1=ALU.add),
             reads=['pred'], writes=['npred'])
        p.op('dve', lambda e: e.tensor_tensor(out=bs['a'][:], in0=bs['mid'][:], in1=bs['pred'][:], op=ALU.mult), reads=['mid', 'pred'], writes=['a'])
        p.op('dve', lambda e: e.tensor_tensor(out=bs['b'][:], in0=bs['mid'][:], in1=bs['npred'][:], op=ALU.mult), reads=['mid', 'npred'], writes=['b'])
        p.op('dve', lambda e: e.tensor_tensor(out=bs['lo'][:], in0=bs['lo'][:], in1=bs['npred'][:], op=ALU.mult), reads=['lo', 'npred'], writes=['lo'])
        p.op('dve', lambda e: e.tensor_tensor(out=bs['lo'][:], in0=bs['lo'][:], in1=bs['a'][:], op=ALU.add), reads=['lo', 'a'], writes=['lo'])
        p.op('dve', lambda e: e.tensor_tensor(out=bs['hi'][:], in0=bs['hi'][:], in1=bs['pred'][:], op=ALU.mult), reads=['hi', 'pred'], writes=['hi'])
        p.op('dve', lambda e: e.tensor_tensor(out=bs['hi'][:], in0=bs['hi'][:], in1=bs['b'][:], op=ALU.add), reads=['hi', 'b'], writes=['hi'])
    for (j, c0, c1) in [(0, 0, TOK), (1, TOK, NO)]:
        p.op('dve', lambda e, j=j, c0=c0, c1=c1: e.scalar_tensor_tensor(out=gsel[:, c0:c1], in0=eO[:, c0:c1], scalar=bs['lo'][:, j:j + 1], in1=eO[:, c0:c1],
                                                                  op0=ALU.is_ge, op1=ALU.mult), reads=['eO', 'lo'], writes=['gsel'])
    p.barrier()

    wg_v = w_gate.rearrange("e (k p) f -> e p k f", p=128)
    wu_v = w_up.rearrange("e (k p) f -> e p k f", p=128)
    h2v = h2T.rearrange("(k p) n -> p k n", p=128)
    cntw = 0
    cntd = 0
    for grp in range(NO // GT):
        g0 = grp * GT
        chunks = [(0, 512), (512, GT)]
        p.dma(h2g[:], h2v[:, :, g0:g0 + GT], writes=['h2g'])
        p.op('dve', lambda e: e.memset(accf[:], 0.0), writes=['acc'])
        for ex in range(NE):
            for ci, (a0, a1) in enumerate(chunks):
                n = a1 - a0
                p.op('pe', lambda e, ex=ex, a0=a0, a1=a1, n=n, g0=g0: e.matmul(pG[:, 0:n], lhsT=selsb[:, ex, :], rhs=gsel[:, g0 + a0:g0 + a1], start=True, stop=True),
                     reads=['sel', 'gsel'], writes=['pG'])
                p.op('act', lambda e, ci=ci, n=n: e.copy(out=gbc[ci][:, 0:n], in_=pG[:, 0:n]), reads=['pG'], writes=['gbc%d' % ci])
            for ft in range(8):
                bi = cntw % 2
                cntw += 1
                p.dma(wgs[:], wg_v[ex, :, :, ft * 128:(ft + 1) * 128], writes=['wgs'])
                p.dma(wus[:], wu_v[ex, :, :, ft * 128:(ft + 1) * 128], writes=['wus'])
                p.dma(wds[:], w_down[ex, ft * 128:(ft + 1) * 128, :], writes=['wds'])
                p.op('pool', lambda e, bi=bi: e.tensor_copy(out=wgb[bi][:], in_=wgs[:]), reads=['wgs'], writes=['wgb%d' % bi])
                p.op('pool', lambda e, bi=bi: e.tensor_copy(out=wub[bi][:], in_=wus[:]), reads=['wus'], writes=['wub%d' % bi])
                p.op('pool', lambda e, ft=ft: e.tensor_copy(out=wdb[:, ft, :], in_=wds[:]), reads=['wds'], writes=['wdb'])
                for ci, (a0, a1) in enumerate(chunks):
                    n = a1 - a0
                    for k in range(16):
                        p.op('pe', lambda e, k=k, bi=bi, ci=ci, n=n, a0=a0, a1=a1: e.matmul(
                            pA[ci][:, 0:n], lhsT=wgb[bi][:, k, :], rhs=h2g[:, k, a0:a1], start=(k == 0), stop=(k == 15)),
                            reads=['wgb%d' % bi, 'h2g'], writes=['pA%d' % ci], inc=(k == 15))
                    for k in range(16):
                        p.op('pe', lambda e, k=k, bi=bi, ci=ci, n=n, a0=a0, a1=a1: e.matmul(
                            pU[ci][:, 0:n], lhsT=wub[bi][:, k, :], rhs=h2g[:, k, a0:a1], start=(k == 0), stop=(k == 15)),
                            reads=['wub%d' % bi, 'h2g'], writes=['pU%d' % ci], inc=(k == 15))
                    p.op('act', lambda e, ci=ci, n=n: e.activation(out=sil[ci][:, 0:n], in_=pA[ci][:, 0:n], func=AF.Silu),
                         reads=['pA%d' % ci], writes=['sil%d' % ci])
                    p.op('dve', lambda e, ci=ci, n=n: e.tensor_tensor(out=sil[ci][:, 0:n], in0=sil[ci][:, 0:n], in1=pU[ci][:, 0:n], op=ALU.mult),
                         reads=['sil%d' % ci, 'pU%d' % ci], writes=['sil%d' % ci])
                    p.op('dve', lambda e, ci=ci, n=n, ft=ft, a0=a0, a1=a1: e.tensor_tensor(out=s3[:, ft, a0:a1], in0=sil[ci][:, 0:n], in1=gbc[ci][:, 0:n], op=ALU.mult),
                         reads=['sil%d' % ci, 'gbc%d' % ci], writes=['s3'])
            for ci, (a0, a1) in enumerate(chunks):
                n = a1 - a0
                for dt in range(16):
                    di = cntd % 2
                    cntd += 1
                    for ft in range(8):
                        p.op('pe', lambda e, ft=ft, dt=dt, di=di, n=n, a0=a0, a1=a1: e.matmul(
                            pD[di][:, 0:n], lhsT=wdb[:, ft, dt * 128:(dt + 1) * 128], rhs=s3[:, ft, a0:a1], start=(ft == 0), stop=(ft == 7)),
                            reads=['wdb', 's3'], writes=['pD%d' % di], inc=(ft == 7))
                    p.op('dve', lambda e, dt=dt, di=di, n=n, a0=a0, a1=a1: e.tensor_tensor(out=acc[:, dt, a0:a1], in0=acc[:, dt, a0:a1], in1=pD[di][:, 0:n], op=ALU.add),
                         reads=['acc', 'pD%d' % di], writes=['acc'])
        srcs = [(0, GT, 0)] if g0 + GT <= TOK else [(0, TOK - g0, 0), (TOK - g0, GT, 1)]
        for dt in range(16):
            xi = dt % 2
            p.dma(xm[xi][:], xmidT[dt * 128:(dt + 1) * 128, g0:g0 + GT], writes=['xm%d' % xi])
            for (a0, a1, src) in srcs:
                p.op('dve', lambda e, dt=dt, xi=xi, a0=a0, a1=a1, src=src: e.scalar_tensor_tensor(
                    out=acc[:, dt, a0:a1], in0=acc[:, dt, a0:a1], scalar=modsb[:, src, 5, dt:dt + 1], in1=xm[xi][:, a0:a1],
                    op0=ALU.mult, op1=ALU.add), reads=['acc', 'xm%d' % xi, 'mod'], writes=['acc'])
            p.dma(xoutT[dt * 128:(dt + 1) * 128, g0:g0 + GT], acc[:, dt, :], reads=['acc'], Q='pool')
            p.op('act', lambda e, dt=dt, xi=xi: e.activation(out=xm[xi][:], in_=acc[:, dt, :], func=AF.Square), reads=['acc'], writes=['xm%d' % xi])
            for ci, (a0, a1) in enumerate(chunks):
                n = a1 - a0
                p.op('pe', lambda e, ci=ci, n=n, a0=a0, a1=a1, dt=dt, xi=xi: e.matmul(pA[ci][:, 0:n], lhsT=onesf[:], rhs=xm[xi][:, a0:a1],
                                                                               start=(dt == 0), stop=(dt == 15)),
                     reads=['xm%d' % xi, 'onesf'], writes=['pA%d' % ci])
        for ci, (a0, a1) in enumerate(chunks):
            n = a1 - a0
            p.op('dve', lambda e, ci=ci, n=n, a0=a0, a1=a1: e.tensor_scalar(out=rstd[:, a0:a1], in0=pA[ci][:, 0:n], scalar1=1.0 / D, scalar2=EPS,
                                                                      op0=ALU.mult, op1=ALU.add), reads=['pA%d' % ci], writes=['rstd'])
        p.op('act', lambda e: e.activation(out=rstd[:], in_=rstd[:], func=AF.Sqrt), reads=['rstd'], writes=['rstd'])
        p.op('dve', lambda e: e.reciprocal(out=rstd[:], in_=rstd[:]), reads=['rstd'], writes=['rstd'])
        for dt in range(16):
            xi = dt % 2
            p.op('dve', lambda e, dt=dt, xi=xi: e.scalar_tensor_tensor(out=xm[xi][:], in0=acc[:, dt, :], scalar=fgsb[:, dt:dt + 1], in1=rstd[:],
                                                                  op0=ALU.mult, op1=ALU.mult), reads=['acc', 'rstd', 'fg'], writes=['xm%d' % xi])
            p.dma(xnT[dt * 128:(dt + 1) * 128, g0:g0 + GT], xm[xi][:], reads=['xm%d' % xi], Q='pool')
    return p.finish()


MH = 6 * D // 2


def build_M():
    p = Prog()
    cT = p.dram('cT', [128, 16, 3], F32)
    w = p.dram('w', [D, MH], F32)
    b = p.dram('b', [1, MH], F32)
    lq = p.dram('lq', [4, 4, 64], F32)
    li = p.dram('li', [4, 1], F32)
    modo = p.dram('modo', [3, MH], F32, kind="ExternalOutput")
    lamo = p.dram('lamo', [4, 1], F32, kind="ExternalOutput")
    cs = p.sb('cs', [128, 16, 3], F32)
    bsb = p.sb('bsb', [3, MH], F32)
    osb = p.sb('osb', [3, MH], F32)
    wch = [p.sb('wch%d' % i, [128, 16, 512], F32) for i in range(2)]
    lqs = p.sb('lqs', [4, 4, 64], F32)
    lis = p.sb('lis', [4, 1], F32)
    pr = p.sb('pr', [4, 2, 64], F32)
    sm = p.sb('sm', [4, 2], F32)
    lam = p.sb('lam', [4, 1], F32)
    ps = [p.ps('ps%d' % i, [128, 512]) for i in range(2)]
    p.dma(cs[:], cT, writes=['cs'])
    p.dma(bsb[:], b[0, :].partition_broadcast(3), writes=['bsb'])
    p.dma(lqs[:], lq, writes=['lqs'])
    p.dma(lis[:], li, writes=['lis'])
    p.op('act', lambda e: e.activation(out=cs[:], in_=cs[:], func=AF.Silu), reads=['cs'], writes=['cs'])
    w_v = w.rearrange("(k p) c -> p k c", p=128)
    for j in range(MH // 512):
        bi = j % 2
        p.dma(wch[bi][:], w_v[:, :, j * 512:(j + 1) * 512], writes=['wch%d' % bi])
        for k in range(16):
            p.op('pe', lambda e, k=k, bi=bi: e.matmul(ps[bi][0:3, :], lhsT=cs[:, k, :], rhs=wch[bi][:, k, :], start=(k == 0), stop=(k == 15)),
                 reads=['cs', 'wch%d' % bi], writes=['ps%d' % bi], inc=(k == 15))
        p.op('dve', lambda e, j=j, bi=bi: e.tensor_tensor(out=osb[:, j * 512:(j + 1) * 512], in0=ps[bi][0:3, :], in1=bsb[:, j * 512:(j + 1) * 512], op=ALU.add),
             reads=['ps%d' % bi, 'bsb'], writes=['osb'])
    p.dma(modo, osb[:], reads=['osb'], Q='pool')
    for j in range(2):
        p.op('dve', lambda e, j=j: e.tensor_tensor(out=pr[:, j, :], in0=lqs[:, 2 * j, :], in1=lqs[:, 2 * j + 1, :], op=ALU.mult), reads=['lqs'], writes=['pr'])
        p.op('dve', lambda e, j=j: e.reduce_sum(out=sm[:, j:j + 1], in_=pr[:, j, :], axis=AX.X), reads=['pr'], writes=['sm'])
    p.op('act', lambda e: e.activation(out=sm[:], in_=sm[:], func=AF.Exp), reads=['sm'], writes=['sm'])
    p.op('dve', lambda e: e.tensor_tensor(out=lam[:], in0=sm[:, 0:1], in1=sm[:, 1:2], op=ALU.subtract), reads=['sm'], writes=['lam'])
    p.op('dve', lambda e: e.tensor_tensor(out=lam[:], in0=lam[:], in1=lis[:], op=ALU.add), reads=['lam', 'lis'], writes=['lam'])
    p.dma(lamo, lam[:], reads=['lam'], Q='pool')
    return p.finish()


_PROGS = {}
N_CORES = 8
LAM_INIT = [0.8 - 0.6 * math.exp(-0.3 * l) for l in range(DEPTH)]


def _prog(name):
    if name not in _PROGS:
        _PROGS[name] = {'A': build_A, 'B': build_B, 'C': build_C, 'M': build_M}[name]()
    return _PROGS[name]


def _run(name, ims):
    import sys as _sys
    import time as _time
    t0 = _time.time()
    prog = _prog(name)
    t1 = _time.time()
    res = run_bass_kernel_spmd(prog, ims, core_ids=list(range(N_CORES)))
    print('[kernel] launch %s build %.1fs run %.1fs' % (name, t1 - t0, _time.time() - t1), file=_sys.stderr, flush=True)
    return res.results


def run_M(W):
    cT = np.ascontiguousarray(_pk(np.stack([W['c'][0], W['c'][1], W['c_ctx']])).transpose(0, 2, 1))
    lq = np.ascontiguousarray(np.stack([W['lambda_q1'], W['lambda_k1'], W['lambda_q2'], W['lambda_k2']], axis=1))
    li = np.array(LAM_INIT, np.float32).reshape(4, 1)
    ims = []
    for i in range(N_CORES):
        l, hf = i // 2, i % 2
        ims.append({'cT': cT, 'w': np.ascontiguousarray(W['w_ada'][l][:, hf * MH:(hf + 1) * MH]),
                    'b': np.ascontiguousarray(W['b_ada'][l][None, hf * MH:(hf + 1) * MH]), 'lq': lq, 'li': li})
    res = _run('M', ims)
    mods = np.zeros((DEPTH, 3, 6, D), np.float32)
    for l in range(DEPTH):
        full = np.concatenate([np.asarray(res[2 * l]['modo']), np.asarray(res[2 * l + 1]['modo'])], axis=1)
        mods[l] = full.reshape(3, 6, D)
    lam = np.asarray(res[0]['lamo']).reshape(4).astype(np.float32)
    return mods, lam


def _halo_fm(xbT, s, xcbT):
    out = np.zeros((D, NT), np.float32)
    lo = s * TOK - HALO
    hi = (s + 1) * TOK + HALO
    a, b = max(lo, 0), min(hi, SEQ)
    out[:, a - lo:a - lo + (b - a)] = xbT[:, a:b]
    out[:, CTX0:CTX0 + CTX] = xcbT
    return out


def _mod_in(mods, l, b):
    return np.ascontiguousarray(np.stack([_pk(mods[l, b]), _pk(mods[l, 2])], axis=1))


def run_A(l, xT, xcT, mods, W):
    ims = []
    for i in range(N_CORES):
        b, s = i // 4, i % 4
        cosT, sinT = _rope_tables(s)
        ims.append({
            'xT': _halo_fm(xT[b], s, xcT[b]), 'mod': _mod_in(mods, l, b), 'g1': _pk(W['norm1_g'][l]), 'w_in': W['w_in'][l],
            'cosT': cosT, 'sinT': sinT, 'perm': _perm(), 'invc': _invcnt(s),
            'valid': np.tile(np.array([[1.0 if s > 0 else 0.0, 1.0 if s < 3 else 0.0]], np.float32), (128, 1)),
            'pool_w': W['pool_w'][l], 'pool_s': np.ascontiguousarray(W['pool_scale'][l].reshape(4, 128).T),
            'conv_w': np.ascontiguousarray(W['conv_w'][l].reshape(3, 4, 128).transpose(2, 1, 0)),
        })
    return _run('A', ims)


def _own_fm(xT, xcT, i):
    b, s = i // 4, i % 4
    return np.ascontiguousarray(np.concatenate([xT[b][:, s * TOK:(s + 1) * TOK], xcT[b]], axis=1))


def run_B(l, resA, xT, xcT, mods, lam, W):
    kTs, vhs = [], []
    for b in range(2):
        qk = [np.asarray(resA[4 * b + s]['qkvT']) for s in range(4)]
        kT = np.concatenate([qk[0][1024:2048, TOK:NO]] + [qk[s][1024:2048, 0:TOK] for s in range(4)], axis=1)
        vT = np.concatenate([qk[0][2048:3072, TOK:NO]] + [qk[s][2048:3072, 0:TOK] for s in range(4)], axis=1)
        v = vT.T
        vh = np.ascontiguousarray(v.reshape(KT, 128, 8, 128).transpose(2, 1, 0, 3).reshape(8, 128, NK))
        kTs.append(np.ascontiguousarray(kT))
        vhs.append(vh)
    sc = np.zeros((128, 3), np.float32)
    sc[:, 0] = lam[l]
    sc[:, 1] = W['subln_g'][l]
    sc[:, 2] = np.float32(1.0 - LAM_INIT[l])
    w_r = np.ascontiguousarray(W['w_router'][l].reshape(16, 128, 16).transpose(1, 0, 2))
    ims = []
    for i in range(N_CORES):
        b = i // 4
        ims.append({'qT': np.ascontiguousarray(np.asarray(resA[i]['qkvT'])[0:1024]), 'kT': kTs[b], 'vh': vhs[b],
                    'mixT': np.asarray(resA[i]['mixT']), 'xT': _own_fm(xT, xcT, i), 'w_out': W['w_out'][l],
                    'mod': _mod_in(mods, l, b), 'g2': _pk(W['norm2_g'][l]), 'sc': sc, 'w_r': w_r})
    return _run('B', ims)


def run_C(l, resB, mods, W):
    sel = np.zeros((16, 16, 128), np.float32)
    for e in range(16):
        sel[e, e, :] = 1.0
    logAs = []
    for b in range(2):
        lg = [np.asarray(resB[4 * b + s]['logT']) for s in range(4)]
        logAs.append(np.ascontiguousarray(np.concatenate([lg[s][:, 0:TOK] for s in range(4)] + [lg[0][:, TOK:NO]], axis=1)))
    ims = []
    for i in range(N_CORES):
        b = i // 4
        ims.append({'logA': logAs[b], 'logO': np.asarray(resB[i]['logT']), 'h2T': np.asarray(resB[i]['h2T']),
                    'xmidT': np.asarray(resB[i]['xmidT']), 'mod': _mod_in(mods, l, b), 'fg': _pk(W['final_g']), 'sel': sel,
                    'w_gate': W['w_gate'][l], 'w_up': W['w_up'][l], 'w_down': W['w_down'][l]})
    return _run('C', ims)


def kernel(**inputs):
    W = {k: np.asarray(v, dtype=np.float32) for k, v in inputs.items()}
    mods, lam = run_M(W)
    xT = [np.ascontiguousarray(W['x'][b].T) for b in range(2)]
    xcT = [np.ascontiguousarray(W['ctx'][b].T) for b in range(2)]
    out = np.zeros((2, SEQ, D), np.float32)
    for l in range(DEPTH):
        resA = run_A(l, xT, xcT, mods, W)
        resB = run_B(l, resA, xT, xcT, mods, lam, W)
        del resA
        resC = run_C(l, resB, mods, W)
        del resB
        for i in range(N_CORES):
            b, s = i // 4, i % 4
            xo = np.asarray(resC[i]['xoutT'])
            xT[b][:, s * TOK:(s + 1) * TOK] = xo[:, 0:TOK]
            if s == 0:
                xcT[b] = np.ascontiguousarray(xo[:, TOK:NO])
            if l == DEPTH - 1:
                out[b, s * TOK:(s + 1) * TOK, :] = np.asarray(resC[i]['xnT'])[:, 0:TOK].T
        del resC
    return out
```

```python
import contextlib
import math
import numpy as np
import ml_dtypes
import concourse.bass as bass
import concourse.mybir as mybir
from concourse.bass_utils import run_bass_kernel_spmd

F32 = mybir.dt.float32
BF16 = mybir.dt.bfloat16
AF = mybir.ActivationFunctionType
ALU = mybir.AluOpType
AX = mybir.AxisListType
NPBF = ml_dtypes.bfloat16
KD = 8

D = 2048
SEQ = 8192
CTX = 256
DEPTH = 4
NE = 16
FF = 1024
EPS = 1e-6
TOK = 2048
HALO = 8
NL = TOK + 2 * HALO
NCX = CTX + 2 * HALO
NT = NL + NCX
LAT0 = HALO
CTX0 = NL + HALO
NO = TOK + CTX


class Prog:
    ENG = ['sp', 'act', 'dve', 'pool', 'pe']

    def __init__(self):
        self.nc = bass.Bass("TRN2", target_bir_lowering=False)
        self.lists = {e: [] for e in self.ENG}
        self.seq = {e: 0 for e in self.ENG}
        self.lastw = {}
        self.readers = {}
        self.seen = {e: {} for e in self.ENG}
        self.dcount = {'sp': 0, 'pool': 0, 'act': 0}
        self.stack = contextlib.ExitStack()
        self.semvals = {}

    def dram(self, name, shape, dt, kind="ExternalInput"):
        return self.nc.dram_tensor(name, list(shape), dt, kind=kind).ap()

    def sb(self, name, shape, dt=F32):
        return self.stack.enter_context(self.nc.sbuf_tensor(name, list(shape), dt))

    def ps(self, name, shape, dt=F32):
        return self.stack.enter_context(self.nc.psum_tensor(name, list(shape), dt))

    def _emit(self, E, fn, reads, writes, ev, inc, extra=()):
        waits = {}

        def need(e2):
            if e2 is None:
                return
            eng, s = e2
            if eng == 'pe' and E == 'pe':
                return
            if self.seen[E].get(eng, 0) >= s:
                return
            waits[eng] = max(waits.get(eng, 0), s)
        for r in reads:
            need(self.lastw.get(r))
        for w in writes:
            need(self.lastw.get(w))
            for eng, s in self.readers.get(w, {}).items():
                need((eng, s))
        for e2 in extra:
            need(e2)
        for eng, s in waits.items():
            self.seen[E][eng] = s
        for r in reads:
            d = self.readers.setdefault(r, {})
            d[ev[0]] = max(d.get(ev[0], 0), ev[1])
        for w in writes:
            self.lastw[w] = ev
            self.readers[w] = {}
        self.semvals[ev[0]] = max(self.semvals.get(ev[0], 0), ev[1])
        self.lists[E].append((list(waits.items()), fn, inc))

    def op(self, E, fn, reads=(), writes=(), inc=True):
        s = self.seq[E] + 1
        if inc:
            self.seq[E] = s
        self._emit(E, fn, reads, writes, (E, s), (E, 1) if inc else None)

    def dma(self, out, in_, reads=(), writes=(), Q='sp'):
        i = self.dcount[Q]
        self.dcount[Q] += 1
        name = 'd%s%d' % (Q, i % KD)
        val = 16 * (i // KD + 1)
        extra = [(name, val - 16)] if val > 16 else []
        self._emit(Q, lambda e: e.dma_start(out=out, in_=in_), reads, writes, (name, val), (name, 16), extra)

    def coll(self, kind, in_, out, reads=(), writes=(), groups=None):
        Q = 'pool'
        i = self.dcount[Q]
        self.dcount[Q] += 1
        name = 'd%s%d' % (Q, i % KD)
        val = 16 * (i // KD + 1)
        extra = [(name, val - 16)] if val > 16 else []
        rg = groups if groups is not None else [list(range(8))]
        self._emit(Q, lambda e: e.collective_compute(kind, ALU.bypass, replica_groups=rg, ins=[in_], outs=[out]),
                   reads, writes, (name, val), (name, 16), extra)

    def barrier(self):
        evs = list(self.semvals.items())
        for E in self.ENG:
            waits = []
            for eng, s in evs:
                if self.seen[E].get(eng, 0) < s:
                    waits.append((eng, s))
                    self.seen[E][eng] = s
            if waits:
                self.lists[E].append((waits, None, None))

    def finish(self):
        nc = self.nc
        self.barrier()
        names = sorted(self.semvals.keys())
        sems = {n: self.stack.enter_context(nc.semaphore(n)) for n in names}
        lists = self.lists
        with nc.Block() as block:
            def mk(E):
                def body(eng):
                    for waits, fn, inc in lists[E]:
                        for (s, v) in waits:
                            eng.wait_ge(sems[s], v)
                        if fn is not None:
                            ins = fn(eng)
                            if inc is not None:
                                ins.then_inc(sems[inc[0]], inc[1])
                return body
            block.sync(mk('sp'))
            block.scalar(mk('act'))
            block.vector(mk('dve'))
            block.gpsimd(mk('pool'))
            block.tensor(mk('pe'))
        self.stack.close()
        return nc


def build_A():
    p = Prog()
    xT = p.dram('xT', [D, NT], F32)
    mod = p.dram('mod', [128, 2, 6, 16], F32)
    g1 = p.dram('g1', [128, 16], F32)
    w_in = p.dram('w_in', [D, 5120], F32)
    cosT = p.dram('cosT', [128, TOK], F32)
    sinT = p.dram('sinT', [128, TOK], F32)
    perm = p.dram('perm', [128, 128], F32)
    invc = p.dram('invc', [4, NT], F32)
    valid = p.dram('valid', [128, 2], F32)
    pool_w = p.dram('pool_w', [4, 128, 128], F32)
    pool_s = p.dram('pool_s', [128, 4], F32)
    conv_w = p.dram('conv_w', [128, 4, 3], F32)
    qkvT = p.dram('qkvT', [3072, NO], BF16, kind="ExternalOutput")
    mixT = p.dram('mixT', [1024, NO], BF16, kind="ExternalOutput")

    hT = p.sb('hT', [128, 16, NT], BF16)
    scr = p.sb('scr', [128, 8192], F32)
    tmpA = p.sb('tmpA', [128, NT], F32)
    tmpB = p.sb('tmpB', [128, NT], F32)
    fb = p.sb('fb', [128, NT], BF16)
    cs = p.sb('cs', [128, 2, 512], F32)
    invb = p.sb('invb', [128, NT], F32)
    ws = p.sb('ws', [128, 16, 128], F32)
    wb = [p.sb('wb%d' % i, [128, 16, 128], BF16) for i in range(2)]
    ob = [p.sb('ob%d' % i, [128, NO], BF16) for i in range(2)]
    modsb = p.sb('modsb', [128, 2, 6, 16], F32)
    g1sb = p.sb('g1sb', [128, 16], F32)
    Asb = p.sb('Asb', [128, 2, 16], F32)
    ones = p.sb('ones', [128, 128], F32)
    permsb = p.sb('permsb', [128, 128], F32)
    validsb = p.sb('validsb', [128, 2], F32)
    pws = p.sb('pws', [128, 4, 128], F32)
    pwb = p.sb('pwb', [128, 4, 128], BF16)
    pssb = p.sb('pssb', [128, 4], F32)
    cwsb = p.sb('cwsb', [128, 4, 3], F32)
    sq = [p.sb('sq%d' % i, [128, 256], F32) for i in range(2)]
    rstd = p.sb('rstd', [128, 256], F32)
    t1 = [p.sb('t1_%d' % i, [128, 256], F32) for i in range(2)]
    pa = [p.ps('pa%d' % i, [128, 512]) for i in range(2)]
    pr = p.ps('pr', [128, 512])
    pp = p.ps('pp', [128, 512])
    pss = p.ps('pss', [128, 512])

    p.dma(modsb[:], mod, writes=['mod'])
    p.dma(g1sb[:], g1, writes=['g1'])
    p.dma(permsb[:], perm, writes=['perm'])
    p.dma(validsb[:], valid, writes=['valid'])
    p.dma(pws[:], pool_w.rearrange("g c d -> c g d"), writes=['pws'])
    p.dma(pssb[:], pool_s, writes=['pssb'])
    p.dma(cwsb[:], conv_w, writes=['cwsb'])
    p.op('dve', lambda e: e.memset(ones[:], 1.0), writes=['ones'])
    p.op('dve', lambda e: e.memset(tmpA[:], 0.0), writes=['tmpA'])
    p.op('dve', lambda e: e.memset(tmpB[:], 0.0), writes=['tmpB'])
    p.op('pool', lambda e: e.tensor_copy(out=pwb[:], in_=pws[:]), reads=['pws'], writes=['pwb'])
    for s in range(2):
        p.op('dve', lambda e, s=s: e.scalar_tensor_tensor(out=Asb[:, s, :], in0=modsb[:, s, 1, :], scalar=1.0,
                                                        in1=g1sb[:], op0=ALU.add, op1=ALU.mult),
             reads=['mod', 'g1'], writes=['A%d' % s])

    xTv = xT.rearrange("(k p) n -> p k n", p=128)
    ch1 = []
    c = 0
    while c < NL:
        n = min(256, NL - c)
        ch1.append((c, c + n, 0))
        c += n
    c = NL
    while c < NT:
        n = min(256, NT - c)
        ch1.append((c, c + n, 1))
        c += n
    for ci, (c0, c1, src) in enumerate(ch1):
        n = c1 - c0
        xi = ci % 2
        xs = scr[:, xi * 4096: xi * 4096 + 16 * 256].rearrange("p (k n) -> p k n", k=16)
        p.dma(xs[:, :, 0:n], xTv[:, :, c0:c1], writes=['xs%d' % xi])
        for k in range(16):
            si = k % 2
            p.op('act', lambda e, k=k, si=si, xs=xs, n=n: e.activation(out=sq[si][:, 0:n], in_=xs[:, k, 0:n], func=AF.Square),
                 reads=['xs%d' % xi], writes=['sq%d' % si])
            p.op('pe', lambda e, k=k, si=si, n=n: e.matmul(pss[:, 0:n], lhsT=ones[:], rhs=sq[si][:, 0:n],
                                                         start=(k == 0), stop=(k == 15)),
                 reads=['sq%d' % si, 'ones'], writes=['pss'])
        p.op('dve', lambda e, n=n: e.tensor_scalar(out=rstd[:, 0:n], in0=pss[:, 0:n], scalar1=1.0 / D, scalar2=EPS,
                                                   op0=ALU.mult, op1=ALU.add), reads=['pss'], writes=['rstd'])
        p.op('act', lambda e, n=n: e.activation(out=rstd[:, 0:n], in_=rstd[:, 0:n], func=AF.Sqrt),
             reads=['rstd'], writes=['rstd'])
        p.op('dve', lambda e, n=n: e.reciprocal(out=rstd[:, 0:n], in_=rstd[:, 0:n]), reads=['rstd'], writes=['rstd'])
        for k in range(16):
            ti = k % 2
            p.op('dve', lambda e, k=k, ti=ti, xs=xs, n=n, src=src: e.scalar_tensor_tensor(
                out=t1[ti][:, 0:n], in0=xs[:, k, 0:n], scalar=Asb[:, src, k:k + 1], in1=rstd[:, 0:n],
                op0=ALU.mult, op1=ALU.mult), reads=['xs%d' % xi, 'rstd', 'A%d' % src], writes=['t1_%d' % ti])
            p.op('act', lambda e, k=k, ti=ti, n=n, src=src, c0=c0, c1=c1: e.activation(
                out=hT[:, k, c0:c1], in_=t1[ti][:, 0:n], func=AF.Identity, bias=modsb[:, src, 0, k:k + 1], scale=1.0),
                reads=['t1_%d' % ti, 'mod'], writes=['hT'])
    p.barrier()

    rows = [scr[:, i * NT:(i + 1) * NT] for i in range(3)]
    w_v = w_in.rearrange("(k p) c -> p k c", p=128)
    ch2 = [(0, 512), (512, 1024), (1024, 1536), (1536, 2048), (2048, NT)]
    own = [(LAT0 + 512 * i, LAT0 + 512 * (i + 1), 512 * i) for i in range(4)] + [(CTX0, CTX0 + CTX, TOK)]
    state = {'tile': 0, 'pa': 0, 'ob': 0}

    def proj(col, r):
        ti = state['tile']
        state['tile'] += 1
        bi = ti % 2
        p.dma(ws[:], w_v[:, :, col:col + 128], writes=['ws'])
        p.op('pool', lambda e: e.tensor_copy(out=wb[bi][:], in_=ws[:]), reads=['ws'], writes=['wb%d' % bi])
        for (c0, c1) in ch2:
            pi = state['pa'] % 2
            state['pa'] += 1
            n = c1 - c0
            for k in range(16):
                p.op('pe', lambda e, k=k, pi=pi, n=n, c0=c0, c1=c1: e.matmul(
                    pa[pi][:, 0:n], lhsT=wb[bi][:, k, :], rhs=hT[:, k, c0:c1], start=(k == 0), stop=(k == 15)),
                    reads=['wb%d' % bi, 'hT'], writes=['pa%d' % pi], inc=(k == 15))
            p.op('act', lambda e, pi=pi, n=n, c0=c0, c1=c1: e.copy(out=rows[r][:, c0:c1], in_=pa[pi][:, 0:n]),
                 reads=['pa%d' % pi], writes=['row%d' % r])

    def zero_halos(buf, key):
        p.op('dve', lambda e: e.tensor_scalar(out=buf[:, 0:HALO], in0=buf[:, 0:HALO], scalar1=validsb[:, 0:1],
                                              scalar2=None, op0=ALU.mult), reads=[key, 'valid'], writes=[key])
        p.op('dve', lambda e: e.tensor_scalar(out=buf[:, NL - HALO:NL], in0=buf[:, NL - HALO:NL],
                                              scalar1=validsb[:, 1:2], scalar2=None, op0=ALU.mult),
             reads=[key, 'valid'], writes=[key])
        p.op('dve', lambda e: e.memset(buf[:, NL:NL + HALO], 0.0), reads=[key], writes=[key])
        p.op('dve', lambda e: e.memset(buf[:, NT - HALO:NT], 0.0), reads=[key], writes=[key])

    def next_ob():
        oi = state['ob'] % 2
        state['ob'] += 1
        return oi

    for j in range(24):
        r = j % 3
        proj(j * 128, r)
        oi = next_ob()
        R = rows[r]
        rk = 'row%d' % r
        ok = 'ob%d' % oi
        if j < 16:
            for c in range(4):
                a0, a1, o0 = own[c]
                p.dma(cs[:, 0, :], cosT[:, o0:o0 + 512], writes=['cs'])
                p.dma(cs[:, 1, :], sinT[:, o0:o0 + 512], writes=['cs'])
                p.op('pe', lambda e, a0=a0, a1=a1, R=R: e.matmul(pr[:], lhsT=permsb[:], rhs=R[:, a0:a1], start=True, stop=True),
                     reads=[rk, 'perm'], writes=['pr'])
                p.op('dve', lambda e, a0=a0, a1=a1, R=R: e.tensor_tensor(out=tmpA[:, 0:512], in0=R[:, a0:a1], in1=cs[:, 0, :], op=ALU.mult),
                     reads=[rk, 'cs'], writes=['tmpA'])
                p.op('dve', lambda e: e.tensor_tensor(out=tmpB[:, 0:512], in0=pr[:], in1=cs[:, 1, :], op=ALU.mult),
                     reads=['pr', 'cs'], writes=['tmpB'])
                p.op('dve', lambda e, o0=o0, oi=oi: e.tensor_tensor(out=ob[oi][:, o0:o0 + 512], in0=tmpA[:, 0:512], in1=tmpB[:, 0:512], op=ALU.add),
                     reads=['tmpA', 'tmpB'], writes=[ok])
        else:
            p.op('act', lambda e, R=R, oi=oi: e.copy(out=ob[oi][:, 0:TOK], in_=R[:, LAT0:LAT0 + TOK]), reads=[rk], writes=[ok])
        p.op('act', lambda e, R=R, oi=oi: e.copy(out=ob[oi][:, TOK:NO], in_=R[:, CTX0:CTX0 + CTX]), reads=[rk], writes=[ok])
        p.dma(qkvT[j * 128:(j + 1) * 128, :], ob[oi][:], reads=[ok], Q='pool')

    for g in range(4):
        r = g % 3
        proj(3072 + g * 128, r)
        R = rows[r]
        rk = 'row%d' % r
        zero_halos(R, rk)
        p.dma(invb[:], invc[g, :].partition_broadcast(128), writes=['invb'])
        p.op('dve', lambda e, R=R: e.tensor_tensor(out=tmpA[:, 1:NT], in0=R[:, 0:NT - 1], in1=R[:, 1:NT], op=ALU.add),
             reads=[rk], writes=['tmpA'])
        S, O, sk, okk = tmpA, tmpB, 'tmpA', 'tmpB'
        if g >= 1:
            p.op('dve', lambda e: e.tensor_tensor(out=tmpB[:, 2:NT - 1], in0=tmpA[:, 1:NT - 2], in1=tmpA[:, 3:NT], op=ALU.add),
                 reads=['tmpA'], writes=['tmpB'])
            S, O, sk, okk = tmpB, tmpA, 'tmpB', 'tmpA'
        if g >= 2:
            p.op('dve', lambda e: e.tensor_tensor(out=tmpA[:, 4:NT - 3], in0=tmpB[:, 2:NT - 5], in1=tmpB[:, 6:NT - 1], op=ALU.add),
                 reads=['tmpB'], writes=['tmpA'])
            S, O, sk, okk = tmpA, tmpB, 'tmpA', 'tmpB'
        if g >= 3:
            p.op('dve', lambda e: e.tensor_tensor(out=tmpB[:, 8:NT - 7], in0=tmpA[:, 4:NT - 11], in1=tmpA[:, 12:NT - 3], op=ALU.add),
                 reads=['tmpA'], writes=['tmpB'])
            S, O, sk, okk = tmpB, tmpA, 'tmpB', 'tmpA'
        p.op('dve', lambda e, S=S, O=O: e.tensor_tensor(out=O[:, 8:NT - 8], in0=S[:, 8:NT - 8], in1=invb[:, 8:NT - 8], op=ALU.mult),
             reads=[sk, 'invb'], writes=[okk])
        p.op('dve', lambda e, O=O, R=R: e.tensor_tensor(out=fb[:, 8:NT - 8], in0=O[:, 8:NT - 8], in1=R[:, 8:NT - 8], op=ALU.subtract),
             reads=[okk, rk], writes=['fb'])
        oi = next_ob()
        ok = 'ob%d' % oi
        for (a0, a1, o0) in own:
            n = a1 - a0
            p.op('pe', lambda e, a0=a0, a1=a1, n=n, g=g: e.matmul(pp[:, 0:n], lhsT=pwb[:, g, :], rhs=fb[:, a0:a1], start=True, stop=True),
                 reads=['fb', 'pwb'], writes=['pp'])
            p.op('dve', lambda e, n=n, o0=o0, oi=oi, g=g: e.tensor_scalar(out=ob[oi][:, o0:o0 + n], in0=pp[:, 0:n], scalar1=pssb[:, g:g + 1],
                                                                   scalar2=None, op0=ALU.mult), reads=['pp', 'pssb'], writes=[ok])
        p.dma(mixT[g * 128:(g + 1) * 128, :], ob[oi][:], reads=[ok], Q='pool')

    for i in range(4):
        proj(3584 + i * 128, 0)
        proj(4096 + i * 128, 1)
        proj(4608 + i * 128, 2)
        p.op('dve', lambda e: e.tensor_tensor(out=tmpA[:], in0=rows[1][:], in1=rows[2][:], op=ALU.mult),
             reads=['row1', 'row2'], writes=['tmpA'])
        zero_halos(tmpA, 'tmpA')
        p.op('dve', lambda e, i=i: e.tensor_scalar(out=tmpB[:, 1:NT - 1], in0=tmpA[:, 0:NT - 2], scalar1=cwsb[:, i, 0:1], scalar2=None, op0=ALU.mult),
             reads=['tmpA', 'cwsb'], writes=['tmpB'])
        p.op('dve', lambda e, i=i: e.scalar_tensor_tensor(out=tmpB[:, 1:NT - 1], in0=tmpA[:, 1:NT - 1], scalar=cwsb[:, i, 1:2], in1=tmpB[:, 1:NT - 1],
                                                         op0=ALU.mult, op1=ALU.add), reads=['tmpA', 'tmpB', 'cwsb'], writes=['tmpB'])
        p.op('dve', lambda e, i=i: e.scalar_tensor_tensor(out=tmpB[:, 1:NT - 1], in0=tmpA[:, 2:NT], scalar=cwsb[:, i, 2:3], in1=tmpB[:, 1:NT - 1],
                                                         op0=ALU.mult, op1=ALU.add), reads=['tmpA', 'tmpB', 'cwsb'], writes=['tmpB'])
        oi = next_ob()
        ok = 'ob%d' % oi
        p.op('dve', lambda e, oi=oi: e.tensor_tensor(out=ob[oi][:, 0:TOK], in0=tmpB[:, LAT0:LAT0 + TOK], in1=rows[0][:, LAT0:LAT0 + TOK], op=ALU.mult),
             reads=['tmpB', 'row0'], writes=[ok])
        p.op('dve', lambda e, oi=oi: e.tensor_tensor(out=ob[oi][:, TOK:NO], in0=tmpB[:, CTX0:CTX0 + CTX], in1=rows[0][:, CTX0:CTX0 + CTX], op=ALU.mult),
             reads=['tmpB', 'row0'], writes=[ok])
        p.dma(mixT[512 + i * 128:512 + (i + 1) * 128, :], ob[oi][:], reads=[ok], Q='pool')
    return p.finish()


def _pk(v):
    v = np.asarray(v, np.float32)
    lead = v.shape[:-1]
    return np.ascontiguousarray(np.moveaxis(v.reshape(lead + (16, 128)), -1, 0))


def _rope_tables(s):
    t = s * TOK + np.arange(TOK)
    row = (t // 64).astype(np.float32)
    col = (t % 64).astype(np.float32)
    inv = (1.0 / (10000.0 ** (np.arange(0, 32, 2, dtype=np.float32) / 32.0))).astype(np.float32)
    ang = np.concatenate([row[:, None] * inv, col[:, None] * inv], axis=-1).astype(np.float32)
    cos = np.cos(ang).astype(np.float32).T
    sin = np.sin(ang).astype(np.float32).T
    cosT = np.tile(cos, (4, 1))
    sinT = np.concatenate([-sin, sin, -sin, sin], axis=0)
    return np.ascontiguousarray(cosT), np.ascontiguousarray(sinT)


def _invcnt(s):
    out = np.zeros((4, NT), np.float32)
    for g, w in enumerate((2, 4, 8, 16)):
        t = s * TOK + np.arange(TOK)
        lo = np.clip(t - w // 2, 0, SEQ)
        hi = np.clip(t + w - w // 2, 0, SEQ)
        out[g, LAT0:LAT0 + TOK] = 1.0 / (hi - lo).astype(np.float32)
        t = np.arange(CTX)
        lo = np.clip(t - w // 2, 0, CTX)
        hi = np.clip(t + w - w // 2, 0, CTX)
        out[g, CTX0:CTX0 + CTX] = 1.0 / (hi - lo).astype(np.float32)
    return out


def _perm():
    P = np.zeros((128, 128), np.float32)
    for i in range(128):
        P[i, i ^ 32] = 1.0
    return P


def _halo_xT(xb, s, xcb):
    out = np.zeros((D, NT), np.float32)
    lo = s * TOK - HALO
    hi = (s + 1) * TOK + HALO
    a, b = max(lo, 0), min(hi, SEQ)
    out[:, a - lo:a - lo + (b - a)] = xb[a:b].T
    out[:, CTX0:CTX0 + CTX] = xcb.T
    return out


def inputs_A(l, core, x, xc, mods, W):
    b, s = core // 4, core % 4
    cosT, sinT = _rope_tables(s)
    mod = np.stack([_pk(mods[l, b]), _pk(mods[l, 2])], axis=1)
    return {
        'xT': _halo_xT(x[b], s, xc[b]),
        'mod': np.ascontiguousarray(mod),
        'g1': _pk(W['norm1_g'][l]),
        'w_in': W['w_in'][l],
        'cosT': cosT, 'sinT': sinT, 'perm': _perm(), 'invc': _invcnt(s),
        'valid': np.tile(np.array([[1.0 if s > 0 else 0.0, 1.0 if s < 3 else 0.0]], np.float32), (128, 1)),
        'pool_w': W['pool_w'][l],
        'pool_s': np.ascontiguousarray(W['pool_scale'][l].reshape(4, 128).T),
        'conv_w': np.ascontiguousarray(W['conv_w'][l].reshape(3, 4, 128).transpose(2, 1, 0)),
    }


NK = SEQ + CTX
KT = NK // 128
CHS = [(0, 512, 0), (512, 1024, 0), (1024, 1536, 0), (1536, 2048, 0), (2048, NO, 1)]


def build_B():
    p = Prog()
    qT = p.dram('qT', [1024, NO], BF16)
    kT = p.dram('kT', [1024, NK], BF16)
    vh = p.dram('vh', [8, 128, NK], BF16)
    mixT = p.dram('mixT', [1024, NO], BF16)
    xT = p.dram('xT', [D, NO], F32)
    w_out = p.dram('w_out', [D, D], F32)
    mod = p.dram('mod', [128, 2, 6, 16], F32)
    g2 = p.dram('g2', [128, 16], F32)
    sc = p.dram('sc', [128, 3], F32)
    w_r = p.dram('w_r', [128, 16, 16], F32)
    xmidT = p.dram('xmidT', [D, NO], F32, kind="ExternalOutput")
    h2T = p.dram('h2T', [D, NO], BF16, kind="ExternalOutput")
    logT = p.dram('logT', [16, NO], F32, kind="ExternalOutput")

    kvf = p.sb('kvf', [128, NK], F32)
    kvb = kvf[:].bitcast(BF16)
    kTh = kvb[:, 0:NK]
    vflat = kvb[:, NK:2 * NK]
    vhs = vflat.rearrange("p (t e) -> p t e", e=128)
    qh = p.sb('qh', [128, NO], BF16)
    ymix = p.sb('ymix', [128, 16, NO], BF16)
    pT = [p.sb('pT%d' % i, [128, 512], BF16) for i in range(2)]
    am = [p.sb('am%d' % i, [128, 512], F32) for i in range(2)]
    rl = p.sb('rl', [128, 512], F32)
    att = p.sb('att', [128, 512], F32)
    sqa = p.sb('sqa', [128, 512], F32)
    rs = p.sb('rs', [128, 512], F32)
    onesb = p.sb('onesb', [128, 128], BF16)
    onesf = p.sb('onesf', [128, 128], F32)
    modsb = p.sb('modsb', [128, 2, 6, 16], F32)
    g2sb = p.sb('g2sb', [128, 16], F32)
    A2 = p.sb('A2', [128, 2, 16], F32)
    scsb = p.sb('scsb', [128, 3], F32)
    sg = p.sb('sg', [128, 1], F32)
    neglam = p.sb('neglam', [128, 1], F32)
    wrsb = p.sb('wrsb', [128, 16, 16], F32)
    ws = p.sb('ws', [128, 16, 128], F32)
    wb = [p.sb('wb%d' % i, [128, 16, 128], BF16) for i in range(2)]
    rstd = p.sb('rstd', [128, NO], F32)
    h2f = [p.sb('h2f%d' % i, [128, NO], F32) for i in range(2)]
    h2b = [p.sb('h2b%d' % i, [128, NO], BF16) for i in range(2)]
    lgs = p.sb('lgs', [16, NO], F32)
    psS = [p.ps('psS%d' % i, [128, 512]) for i in range(2)]
    psO = [p.ps('psO%d' % i, [128, 512]) for i in range(2)]
    psL = [p.ps('psL%d' % i, [128, 512]) for i in range(2)]
    psN = p.ps('psN', [128, 512])

    p.dma(modsb[:], mod, writes=['mod'])
    p.dma(g2sb[:], g2, writes=['g2'])
    p.dma(scsb[:], sc, writes=['sc'])
    p.dma(wrsb[:], w_r, writes=['wr'])
    for i in range(8):
        p.dma(ymix[:, 8 + i, :], mixT[i * 128:(i + 1) * 128, :], writes=['ymixp'])
    p.op('dve', lambda e: e.memset(onesb[:], 1.0), writes=['onesb'])
    p.op('dve', lambda e: e.memset(onesf[:], 1.0), writes=['onesf'])
    p.op('dve', lambda e: e.tensor_tensor(out=sg[:], in0=scsb[:, 1:2], in1=scsb[:, 2:3], op=ALU.mult), reads=['sc'], writes=['sg'])
    p.op('dve', lambda e: e.tensor_scalar(out=neglam[:], in0=scsb[:, 0:1], scalar1=-1.0, scalar2=None, op0=ALU.mult),
         reads=['sc'], writes=['neglam'])
    for s in range(2):
        p.op('dve', lambda e, s=s: e.scalar_tensor_tensor(out=A2[:, s, :], in0=modsb[:, s, 4, :], scalar=1.0,
                                                        in1=g2sb[:], op0=ALU.add, op1=ALU.mult),
             reads=['mod', 'g2'], writes=['A2'])

    for h in range(8):
        p.dma(kTh, kT[h * 128:(h + 1) * 128, :], writes=['kTh'])
        p.dma(vflat, vh[h], writes=['vhs'])
        p.dma(qh[:], qT[h * 128:(h + 1) * 128, :], writes=['qh'])
        tiles = []
        for (q0, q1, src) in CHS:
            nkt = KT if src == 0 else CTX // 128
            for m in range(2):
                for kt in range(nkt):
                    tiles.append((q0, q1, src, m, kt, nkt))

        def emit_qk(i):
            q0, q1, src, m, kt, nkt = tiles[i]
            si = i % 2
            n = q1 - q0
            p.op('pe', lambda e, si=si, m=m, kt=kt, n=n, q0=q0, q1=q1: e.matmul(
                psS[si][:, 0:n], lhsT=kTh[m * 64:(m + 1) * 64, kt * 128:(kt + 1) * 128],
                rhs=qh[m * 64:(m + 1) * 64, q0:q1], start=True, stop=True),
                reads=['kTh', 'qh'], writes=['psS%d' % si])
        emit_qk(0)
        for i, (q0, q1, src, m, kt, nkt) in enumerate(tiles):
            n = q1 - q0
            si = i % 2
            p.op('act', lambda e, si=si, n=n: e.activation(out=pT[si][:, 0:n], in_=psS[si][:, 0:n], func=AF.Exp, scale=0.125),
                 reads=['psS%d' % si], writes=['pT%d' % si])
            if i + 1 < len(tiles):
                emit_qk(i + 1)
            p.op('pe', lambda e, si=si, m=m, kt=kt, n=n, nkt=nkt: e.matmul(
                psO[m][:, 0:n], lhsT=vhs[:, kt, :], rhs=pT[si][:, 0:n], start=(kt == 0), stop=(kt == nkt - 1)),
                reads=['vhs', 'pT%d' % si], writes=['psO%d' % m], inc=False)
            p.op('pe', lambda e, si=si, m=m, kt=kt, n=n, nkt=nkt: e.matmul(
                psL[m][:, 0:n], lhsT=onesb[:], rhs=pT[si][:, 0:n], start=(kt == 0), stop=(kt == nkt - 1)),
                reads=['onesb', 'pT%d' % si], writes=['psL%d' % m])
            if kt == nkt - 1:
                p.op('dve', lambda e, m=m, n=n: e.reciprocal(out=rl[:, 0:n], in_=psL[m][:, 0:n]), reads=['psL%d' % m], writes=['rl'])
                p.op('dve', lambda e, m=m, n=n: e.tensor_tensor(out=am[m][:, 0:n], in0=psO[m][:, 0:n], in1=rl[:, 0:n], op=ALU.mult),
                     reads=['psO%d' % m, 'rl'], writes=['am%d' % m])
            if kt == nkt - 1 and m == 1:
                p.op('dve', lambda e, n=n: e.scalar_tensor_tensor(out=att[:, 0:n], in0=am[1][:, 0:n], scalar=neglam[:, 0:1], in1=am[0][:, 0:n],
                                                               op0=ALU.mult, op1=ALU.add), reads=['am0', 'am1', 'neglam'], writes=['att'])
                p.op('act', lambda e, n=n: e.activation(out=sqa[:, 0:n], in_=att[:, 0:n], func=AF.Square), reads=['att'], writes=['sqa'])
                p.op('pe', lambda e, n=n: e.matmul(psN[:, 0:n], lhsT=onesf[:], rhs=sqa[:, 0:n], start=True, stop=True),
                     reads=['sqa', 'onesf'], writes=['psN'])
                p.op('dve', lambda e, n=n: e.tensor_scalar(out=rs[:, 0:n], in0=psN[:, 0:n], scalar1=1.0 / 128, scalar2=EPS, op0=ALU.mult, op1=ALU.add),
                     reads=['psN'], writes=['rs'])
                p.op('act', lambda e, n=n: e.activation(out=rs[:, 0:n], in_=rs[:, 0:n], func=AF.Sqrt), reads=['rs'], writes=['rs'])
                p.op('dve', lambda e, n=n: e.reciprocal(out=rs[:, 0:n], in_=rs[:, 0:n]), reads=['rs'], writes=['rs'])
                p.op('dve', lambda e, n=n, h=h, q0=q0, q1=q1: e.scalar_tensor_tensor(out=ymix[:, h, q0:q1], in0=att[:, 0:n], scalar=sg[:, 0:1], in1=rs[:, 0:n],
                                                                               op0=ALU.mult, op1=ALU.mult), reads=['att', 'rs', 'sg'], writes=['ymixa'])
    p.barrier()

    xr = [kvf[:, 0:NO], kvf[:, NO:2 * NO]]
    sqb = kvf[:, 2 * NO:3 * NO]
    pq = [psO[0], psO[1], psL[0], psL[1], psN]
    wo_v = w_out.rearrange("(m p) c -> p m c", p=128)
    pac = 0
    for dt in range(16):
        bi = dt % 2
        xi = dt % 2
        p.dma(ws[:], wo_v[:, :, dt * 128:(dt + 1) * 128], writes=['ws'])
        p.op('pool', lambda e, bi=bi: e.tensor_copy(out=wb[bi][:], in_=ws[:]), reads=['ws'], writes=['wb%d' % bi])
        p.dma(xr[xi], xT[dt * 128:(dt + 1) * 128, :], writes=['xr%d' % xi])
        for ci, (c0, c1, src) in enumerate(CHS):
            n = c1 - c0
            pi = pac % 2
            pac += 1
            for m in range(16):
                p.op('pe', lambda e, m=m, bi=bi, pi=pi, n=n, c0=c0, c1=c1: e.matmul(
                    psS[pi][:, 0:n], lhsT=wb[bi][:, m, :], rhs=ymix[:, m, c0:c1], start=(m == 0), stop=(m == 15)),
                    reads=['wb%d' % bi, 'ymixa', 'ymixp'], writes=['psS%d' % pi], inc=(m == 15))
            p.op('dve', lambda e, pi=pi, n=n, c0=c0, c1=c1, src=src, dt=dt, xi=xi: e.scalar_tensor_tensor(
                out=xr[xi][:, c0:c1], in0=psS[pi][:, 0:n], scalar=modsb[:, src, 2, dt:dt + 1], in1=xr[xi][:, c0:c1],
                op0=ALU.mult, op1=ALU.add), reads=['psS%d' % pi, 'xr%d' % xi, 'mod'], writes=['xr%d' % xi])
        p.op('act', lambda e, xi=xi: e.activation(out=sqb, in_=xr[xi], func=AF.Square), reads=['xr%d' % xi], writes=['sqb'])
        for ci, (c0, c1, src) in enumerate(CHS):
            n = c1 - c0
            p.op('pe', lambda e, ci=ci, n=n, c0=c0, c1=c1, dt=dt: e.matmul(pq[ci][:, 0:n], lhsT=onesf[:], rhs=sqb[:, c0:c1],
                                                                     start=(dt == 0), stop=(dt == 15)),
                 reads=['sqb', 'onesf'], writes=['pq%d' % ci])
        p.dma(xmidT[dt * 128:(dt + 1) * 128, :], xr[xi], reads=['xr%d' % xi], writes=['xmid%d' % dt], Q='pool')
    for ci, (c0, c1, src) in enumerate(CHS):
        n = c1 - c0
        p.op('dve', lambda e, ci=ci, n=n, c0=c0, c1=c1: e.tensor_scalar(out=rstd[:, c0:c1], in0=pq[ci][:, 0:n], scalar1=1.0 / D, scalar2=EPS,
                                                                  op0=ALU.mult, op1=ALU.add), reads=['pq%d' % ci], writes=['rstd'])
    p.op('act', lambda e: e.activation(out=rstd[:], in_=rstd[:], func=AF.Sqrt), reads=['rstd'], writes=['rstd'])
    p.op('dve', lambda e: e.reciprocal(out=rstd[:], in_=rstd[:]), reads=['rstd'], writes=['rstd'])
    for k in range(16):
        xi = k % 2
        hi = k % 2
        p.dma(xr[xi], xmidT[k * 128:(k + 1) * 128, :], reads=['xmid%d' % k], writes=['xr%d' % xi])
        for (c0, c1, src) in [(0, TOK, 0), (TOK, NO, 1)]:
            p.op('dve', lambda e, xi=xi, c0=c0, c1=c1, src=src, k=k: e.scalar_tensor_tensor(
                out=xr[xi][:, c0:c1], in0=xr[xi][:, c0:c1], scalar=A2[:, src, k:k + 1], in1=rstd[:, c0:c1],
                op0=ALU.mult, op1=ALU.mult), reads=['xr%d' % xi, 'rstd', 'A2'], writes=['xr%d' % xi])
            p.op('act', lambda e, xi=xi, hi=hi, c0=c0, c1=c1, src=src, k=k: e.activation(
                out=h2f[hi][:, c0:c1], in_=xr[xi][:, c0:c1], func=AF.Identity, bias=modsb[:, src, 3, k:k + 1], scale=1.0),
                reads=['xr%d' % xi, 'mod'], writes=['h2f%d' % hi])
        p.op('pool', lambda e, hi=hi: e.tensor_copy(out=h2b[hi][:], in_=h2f[hi][:]), reads=['h2f%d' % hi], writes=['h2b%d' % hi])
        p.dma(h2T[k * 128:(k + 1) * 128, :], h2b[hi][:], reads=['h2b%d' % hi], Q='pool')
        for ci, (c0, c1, src) in enumerate(CHS):
            n = c1 - c0
            p.op('pe', lambda e, ci=ci, n=n, c0=c0, c1=c1, k=k, hi=hi: e.matmul(pq[ci][0:16, 0:n], lhsT=wrsb[:, k, :], rhs=h2f[hi][:, c0:c1],
                                                                          start=(k == 0), stop=(k == 15)),
                 reads=['h2f%d' % hi, 'wr', 'rstd'], writes=['pq%d' % ci])
    for ci, (c0, c1, src) in enumerate(CHS):
        n = c1 - c0
        p.op('act', lambda e, ci=ci, n=n, c0=c0, c1=c1: e.copy(out=lgs[:, c0:c1], in_=pq[ci][0:16, 0:n]), reads=['pq%d' % ci], writes=['lgs'])
    p.dma(logT, lgs[:], reads=['lgs'], Q='pool')
    return p.finish()


NA = SEQ + CTX
GT = 768
NBIS = 34


def build_C():
    p = Prog()
    logA = p.dram('logA', [16, NA], F32)
    logO = p.dram('logO', [16, NO], F32)
    h2T = p.dram('h2T', [D, NO], BF16)
    xmidT = p.dram('xmidT', [D, NO], F32)
    mod = p.dram('mod', [128, 2, 6, 16], F32)
    fg = p.dram('fg', [128, 16], F32)
    sel = p.dram('sel', [16, 16, 128], F32)
    w_gate = p.dram('w_gate', [NE, D, FF], F32)
    w_up = p.dram('w_up', [NE, D, FF], F32)
    w_down = p.dram('w_down', [NE, FF, D], F32)
    xoutT = p.dram('xoutT', [D, NO], F32, kind="ExternalOutput")
    xnT = p.dram('xnT', [D, NO], F32, kind="ExternalOutput")

    accf = p.sb('accf', [128, 16 * GT], F32)
    acc = accf[:].rearrange("p (k n) -> p k n", k=16)
    cmp = accf[0:16, 0:SEQ]
    wdf = p.sb('wdf', [128, NA], F32)
    eA = wdf[0:16, :]
    wdb = wdf[:].bitcast(BF16)[:, 0:8 * D].rearrange("p (f d) -> p f d", f=8)
    h2g = p.sb('h2g', [128, 16, GT], BF16)
    s3 = p.sb('s3', [128, 8, GT], BF16)
    wgs = p.sb('wgs', [128, 16, 128], F32)
    wus = p.sb('wus', [128, 16, 128], F32)
    wds = p.sb('wds', [128, D], F32)
    wgb = [p.sb('wgb%d' % i, [128, 16, 128], BF16) for i in range(2)]
    wub = [p.sb('wub%d' % i, [128, 16, 128], BF16) for i in range(2)]
    gbc = [p.sb('gbc%d' % i, [128, 512], F32) for i in range(2)]
    sil = [p.sb('sil%d' % i, [128, 512], F32) for i in range(2)]
    eO = p.sb('eO', [16, NO], F32)
    gsel = p.sb('gsel', [16, NO], F32)
    rc = p.sb('rc', [16, 512], F32)
    ones16 = p.sb('ones16', [16, 16], F32)
    onesf = p.sb('onesf', [128, 128], F32)
    selsb = p.sb('selsb', [16, 16, 128], F32)
    modsb = p.sb('modsb', [128, 2, 6, 16], F32)
    fgsb = p.sb('fgsb', [128, 16], F32)
    xm = [p.sb('xm%d' % i, [128, GT], F32) for i in range(2)]
    rstd = p.sb('rstd', [128, GT], F32)
    bs = {nm: p.sb('bs_' + nm, [16, 2], F32) for nm in ['lo', 'hi', 'mid', 'cnt', 'pred', 'npred', 'a', 'b']}
    pA = [p.ps('pA%d' % i, [128, 512]) for i in range(2)]
    pU = [p.ps('pU%d' % i, [128, 512]) for i in range(2)]
    pD = [p.ps('pD%d' % i, [128, 512]) for i in range(2)]
    pG = p.ps('pG', [128, 512])

    p.dma(eA, logA, writes=['eA'])
    p.dma(eO[:], logO, writes=['eO'])
    p.dma(modsb[:], mod, writes=['mod'])
    p.dma(fgsb[:], fg, writes=['fg'])
    p.dma(selsb[:], sel, writes=['sel'])
    p.op('dve', lambda e: e.memset(ones16[:], 1.0), writes=['ones16'])
    p.op('dve', lambda e: e.memset(onesf[:], 1.0), writes=['onesf'])

    def softmax_cols(buf, key, ncols):
        p.op('act', lambda e: e.activation(out=buf, in_=buf, func=AF.Exp), reads=[key], writes=[key])
        c = 0
        while c < ncols:
            n = min(512, ncols - c)
            p.op('pe', lambda e, c=c, n=n: e.matmul(pG[0:16, 0:n], lhsT=ones16[:], rhs=buf[:, c:c + n], start=True, stop=True),
                 reads=[key, 'ones16'], writes=['pG'])
            p.op('dve', lambda e, n=n: e.reciprocal(out=rc[:, 0:n], in_=pG[0:16, 0:n]), reads=['pG'], writes=['rc'])
            p.op('dve', lambda e, c=c, n=n: e.tensor_tensor(out=buf[:, c:c + n], in0=buf[:, c:c + n], in1=rc[:, 0:n], op=ALU.mult),
                 reads=[key, 'rc'], writes=[key])
            c += n
    softmax_cols(eA, 'eA', NA)
    softmax_cols(eO[:], 'eO', NO)

    p.op('dve', lambda e: e.memset(bs['lo'][:], 0.0), writes=['lo'])
    p.op('dve', lambda e: e.memset(bs['hi'][:], 1.5), writes=['hi'])
    sets = [(0, 0, SEQ, 2 * SEQ // NE), (1, SEQ, NA, 2 * CTX // NE)]
    for it in range(NBIS):
        p.op('dve', lambda e: e.tensor_tensor(out=bs['mid'][:], in0=bs['lo'][:], in1=bs['hi'][:], op=ALU.add), reads=['lo', 'hi'], writes=['mid'])
        p.op('dve', lambda e: e.tensor_scalar(out=bs['mid'][:], in0=bs['mid'][:], scalar1=0.5, scalar2=None, op0=ALU.mult), reads=['mid'], writes=['mid'])
        for (j, c0, c1, kk) in sets:
            n = c1 - c0
            p.op('dve', lambda e, j=j, c0=c0, c1=c1, n=n: e.tensor_scalar(out=cmp[:, 0:n], in0=eA[:, c0:c1], scalar1=bs['mid'][:, j:j + 1], scalar2=None,
                                                                    op0=ALU.is_ge), reads=['eA', 'mid'], writes=['cmp'])
            p.op('dve', lambda e, j=j, n=n: e.reduce_sum(out=bs['cnt'][:, j:j + 1], in_=cmp[:, 0:n], axis=AX.X), reads=['cmp'], writes=['cnt'])
            p.op('dve', lambda e, j=j, kk=kk: e.tensor_scalar(out=bs['pred'][:, j:j + 1], in0=bs['cnt'][:, j:j + 1], scalar1=kk - 0.5, scalar2=None,
                                                            op0=ALU.is_ge), reads=['cnt'], writes=['pred'])
        p.op('dve', lambda e: e.tensor_scalar(out=bs['npred'][:], in0=bs['pred'][:], scalar1=-1.0, scalar2=1.0, op0=ALU.mult, op1=ALU.add),
             reads=['pred'], writes=['npred'])
        p.op('dve', lambda e: e.tensor_tensor(out=bs['a'][:], in0=bs['mid'][:], in1=bs['pred'][:], op=ALU.mult), reads=['mid', 'pred'], writes=['a'])
        p.op('dve', lambda e: e.tensor_tensor(out=bs['b'][:], in0=bs['mid'][:], in1=bs['npred'][:], op=ALU.mult), reads=['mid', 'npred'], writes=['b'])
        p.op('dve', lambda e: e.tensor_tensor(out=bs['lo'][:], in0=bs['lo'][:], in1=bs['npred'][:], op=ALU.mult), reads=['lo', 'npred'], writes=['lo'])
        p.op('dve', lambda e: e.tensor_tensor(out=bs['lo'][:], in0=bs['lo'][:], in1=bs['a'][:], op=ALU.add), reads=['lo', 'a'], writes=['lo'])
        p.op('dve', lambda e: e.tensor_tensor(out=bs['hi'][:], in0=bs['hi'][:], in1=bs['pred'][:], op=ALU.mult), reads=['hi', 'pred'], writes=['hi'])
        p.op('dve', lambda e: e.tensor_tensor(out=bs['hi'][:], in0=bs['hi'][:], in1=bs['b'][:], op=ALU.add), reads=['hi', 'b'], writes=['hi'])
    for (j, c0, c1) in [(0, 0, TOK), (1, TOK, NO)]:
        p.op('dve', lambda e, j=j, c0=c0, c1=c1: e.scalar_tensor_tensor(out=gsel[:, c0:c1], in0=eO[:, c0:c1], scalar=bs['lo'][:, j:j + 1], in1=eO[:, c0:c1],
                                                                  op0=ALU.is_ge, op1=ALU.mult), reads=['eO', 'lo'], writes=['gsel'])
    p.barrier()

    wg_v = w_gate.rearrange("e (k p) f -> e p k f", p=128)
    wu_v = w_up.rearrange("e (k p) f -> e p k f", p=128)
    h2v = h2T.rearrange("(k p) n -> p k n", p=128)
    cntw = 0
    cntd = 0
    for grp in range(NO // GT):
        g0 = grp * GT
        chunks = [(0, 512), (512, GT)]
        p.dma(h2g[:], h2v[:, :, g0:g0 + GT], writes=['h2g'])
        p.op('dve', lambda e: e.memset(accf[:], 0.0), writes=['acc'])
        for ex in range(NE):
            for ci, (a0, a1) in enumerate(chunks):
                n = a1 - a0
                p.op('pe', lambda e, ex=ex, a0=a0, a1=a1, n=n, g0=g0: e.matmul(pG[:, 0:n], lhsT=selsb[:, ex, :], rhs=gsel[:, g0 + a0:g0 + a1], start=True, stop=True),
                     reads=['sel', 'gsel'], writes=['pG'])
                p.op('act', lambda e, ci=ci, n=n: e.copy(out=gbc[ci][:, 0:n], in_=pG[:, 0:n]), reads=['pG'], writes=['gbc%d' % ci])
            for ft in range(8):
                bi = cntw % 2
                cntw += 1
                p.dma(wgs[:], wg_v[ex, :, :, ft * 128:(ft + 1) * 128], writes=['wgs'])
                p.dma(wus[:], wu_v[ex, :, :, ft * 128:(ft + 1) * 128], writes=['wus'])
                p.dma(wds[:], w_down[ex, ft * 128:(ft + 1) * 128, :], writes=['wds'])
                p.op('pool', lambda e, bi=bi: e.tensor_copy(out=wgb[bi][:], in_=wgs[:]), reads=['wgs'], writes=['wgb%d' % bi])
                p.op('pool', lambda e, bi=bi: e.tensor_copy(out=wub[bi][:], in_=wus[:]), reads=['wus'], writes=['wub%d' % bi])
                p.op('pool', lambda e, ft=ft: e.tensor_copy(out=wdb[:, ft, :], in_=wds[:]), reads=['wds'], writes=['wdb'])
                for ci, (a0, a1) in enumerate(chunks):
                    n = a1 - a0
                    for k in range(16):
                        p.op('pe', lambda e, k=k, bi=bi, ci=ci, n=n, a0=a0, a1=a1: e.matmul(
                            pA[ci][:, 0:n], lhsT=wgb[bi][:, k, :], rhs=h2g[:, k, a0:a1], start=(k == 0), stop=(k == 15)),
                            reads=['wgb%d' % bi, 'h2g'], writes=['pA%d' % ci], inc=(k == 15))
                    for k in range(16):
                        p.op('pe', lambda e, k=k, bi=bi, ci=ci, n=n, a0=a0, a1=a1: e.matmul(
                            pU[ci][:, 0:n], lhsT=wub[bi][:, k, :], rhs=h2g[:, k, a0:a1], start=(k == 0), stop=(k == 15)),
                            reads=['wub%d' % bi, 'h2g'], writes=['pU%d' % ci], inc=(k == 15))
                    p.op('act', lambda e, ci=ci, n=n: e.activation(out=sil[ci][:, 0:n], in_=pA[ci][:, 0:n], func=AF.Silu),
                         reads=['pA%d' % ci], writes=['sil%d' % ci])
                    p.op('dve', lambda e, ci=ci, n=n: e.tensor_tensor(out=sil[ci][:, 0:n], in0=sil[ci][:, 0:n], in1=pU[ci][:, 0:n], op=ALU.mult),
                         reads=['sil%d' % ci, 'pU%d' % ci], writes=['sil%d' % ci])
                    p.op('dve', lambda e, ci=ci, n=n, ft=ft, a0=a0, a1=a1: e.tensor_tensor(out=s3[:, ft, a0:a1], in0=sil[ci][:, 0:n], in1=gbc[ci][:, 0:n], op=ALU.mult),
                         reads=['sil%d' % ci, 'gbc%d' % ci], writes=['s3'])
            for ci, (a0, a1) in enumerate(chunks):
                n = a1 - a0
                for dt in range(16):
                    di = cntd % 2
                    cntd += 1
                    for ft in range(8):
                        p.op('pe', lambda e, ft=ft, dt=dt, di=di, n=n, a0=a0, a1=a1: e.matmul(
                            pD[di][:, 0:n], lhsT=wdb[:, ft, dt * 128:(dt + 1) * 128], rhs=s3[:, ft, a0:a1], start=(ft == 0), stop=(ft == 7)),
                            reads=['wdb', 's3'], writes=['pD%d' % di], inc=(ft == 7))
                    p.op('dve', lambda e, dt=dt, di=di, n=n, a0=a0, a1=a1: e.tensor_tensor(out=acc[:, dt, a0:a1], in0=acc[:, dt, a0:a1], in1=pD[di][:, 0:n], op=ALU.add),
                         reads=['acc', 'pD%d' % di], writes=['acc'])
        srcs = [(0, GT, 0)] if g0 + GT <= TOK else [(0, TOK - g0, 0), (TOK - g0, GT, 1)]
        for dt in range(16):
            xi = dt % 2
            p.dma(xm[xi][:], xmidT[dt * 128:(dt + 1) * 128, g0:g0 + GT], writes=['xm%d' % xi])
            for (a0, a1, src) in srcs:
                p.op('dve', lambda e, dt=dt, xi=xi, a0=a0, a1=a1, src=src: e.scalar_tensor_tensor(
                    out=acc[:, dt, a0:a1], in0=acc[:, dt, a0:a1], scalar=modsb[:, src, 5, dt:dt + 1], in1=xm[xi][:, a0:a1],
                    op0=ALU.mult, op1=ALU.add), reads=['acc', 'xm%d' % xi, 'mod'], writes=['acc'])
            p.dma(xoutT[dt * 128:(dt + 1) * 128, g0:g0 + GT], acc[:, dt, :], reads=['acc'], Q='pool')
            p.op('act', lambda e, dt=dt, xi=xi: e.activation(out=xm[xi][:], in_=acc[:, dt, :], func=AF.Square), reads=['acc'], writes=['xm%d' % xi])
            for ci, (a0, a1) in enumerate(chunks):
                n = a1 - a0
                p.op('pe', lambda e, ci=ci, n=n, a0=a0, a1=a1, dt=dt, xi=xi: e.matmul(pA[ci][:, 0:n], lhsT=onesf[:], rhs=xm[xi][:, a0:a1],
                                                                               start=(dt == 0), stop=(dt == 15)),
                     reads=['xm%d' % xi, 'onesf'], writes=['pA%d' % ci])
        for ci, (a0, a1) in enumerate(chunks):
            n = a1 - a0
            p.op('dve', lambda e, ci=ci, n=n, a0=a0, a1=a1: e.tensor_scalar(out=rstd[:, a0:a1], in0=pA[ci][:, 0:n], scalar1=1.0 / D, scalar2=EPS,
                                                                      op0=ALU.mult, op1=ALU.add), reads=['pA%d' % ci], writes=['rstd'])
        p.op('act', lambda e: e.activation(out=rstd[:], in_=rstd[:], func=AF.Sqrt), reads=['rstd'], writes=['rstd'])
        p.op('dve', lambda e: e.reciprocal(out=rstd[:], in_=rstd[:]), reads=['rstd'], writes=['rstd'])
        for dt in range(16):
            xi = dt % 2
            p.op('dve', lambda e, dt=dt, xi=xi: e.scalar_tensor_tensor(out=xm[xi][:], in0=acc[:, dt, :], scalar=fgsb[:, dt:dt + 1], in1=rstd[:],
                                                                  op0=ALU.mult, op1=ALU.mult), reads=['acc', 'rstd', 'fg'], writes=['xm%d' % xi])
            p.dma(xnT[dt * 128:(dt + 1) * 128, g0:g0 + GT], xm[xi][:], reads=['xm%d' % xi], Q='pool')
    return p.finish()


MH = 6 * D // 2


def build_M():
    p = Prog()
    cT = p.dram('cT', [128, 16, 3], F32)
    w = p.dram('w', [D, MH], F32)
    b = p.dram('b', [1, MH], F32)
    lq = p.dram('lq', [4, 4, 64], F32)
    li = p.dram('li', [4, 1], F32)
    modo = p.dram('modo', [3, MH], F32, kind="ExternalOutput")
    lamo = p.dram('lamo', [4, 1], F32, kind="ExternalOutput")
    cs = p.sb('cs', [128, 16, 3], F32)
    bsb = p.sb('bsb', [3, MH], F32)
    osb = p.sb('osb', [3, MH], F32)
    wch = [p.sb('wch%d' % i, [128, 16, 512], F32) for i in range(2)]
    lqs = p.sb('lqs', [4, 4, 64], F32)
    lis = p.sb('lis', [4, 1], F32)
    pr = p.sb('pr', [4, 2, 64], F32)
    sm = p.sb('sm', [4, 2], F32)
    lam = p.sb('lam', [4, 1], F32)
    ps = [p.ps('ps%d' % i, [128, 512]) for i in range(2)]
    p.dma(cs[:], cT, writes=['cs'])
    p.dma(bsb[:], b[0, :].partition_broadcast(3), writes=['bsb'])
    p.dma(lqs[:], lq, writes=['lqs'])
    p.dma(lis[:], li, writes=['lis'])
    p.op('act', lambda e: e.activation(out=cs[:], in_=cs[:], func=AF.Silu), reads=['cs'], writes=['cs'])
    w_v = w.rearrange("(k p) c -> p k c", p=128)
    for j in range(MH // 512):
        bi = j % 2
        p.dma(wch[bi][:], w_v[:, :, j * 512:(j + 1) * 512], writes=['wch%d' % bi])
        for k in range(16):
            p.op('pe', lambda e, k=k, bi=bi: e.matmul(ps[bi][0:3, :], lhsT=cs[:, k, :], rhs=wch[bi][:, k, :], start=(k == 0), stop=(k == 15)),
                 reads=['cs', 'wch%d' % bi], writes=['ps%d' % bi], inc=(k == 15))
        p.op('dve', lambda e, j=j, bi=bi: e.tensor_tensor(out=osb[:, j * 512:(j + 1) * 512], in0=ps[bi][0:3, :], in1=bsb[:, j * 512:(j + 1) * 512], op=ALU.add),
             reads=['ps%d' % bi, 'bsb'], writes=['osb'])
    p.dma(modo, osb[:], reads=['osb'], Q='pool')
    for j in range(2):
        p.op('dve', lambda e, j=j: e.tensor_tensor(out=pr[:, j, :], in0=lqs[:, 2 * j, :], in1=lqs[:, 2 * j + 1, :], op=ALU.mult), reads=['lqs'], writes=['pr'])
        p.op('dve', lambda e, j=j: e.reduce_sum(out=sm[:, j:j + 1], in_=pr[:, j, :], axis=AX.X), reads=['pr'], writes=['sm'])
    p.op('act', lambda e: e.activation(out=sm[:], in_=sm[:], func=AF.Exp), reads=['sm'], writes=['sm'])
    p.op('dve', lambda e: e.tensor_tensor(out=lam[:], in0=sm[:, 0:1], in1=sm[:, 1:2], op=ALU.subtract), reads=['sm'], writes=['lam'])
    p.op('dve', lambda e: e.tensor_tensor(out=lam[:], in0=lam[:], in1=lis[:], op=ALU.add), reads=['lam', 'lis'], writes=['lam'])
    p.dma(lamo, lam[:], reads=['lam'], Q='pool')
    return p.finish()


_PROGS = {}
N_CORES = 8
LAM_INIT = [0.8 - 0.6 * math.exp(-0.3 * l) for l in range(DEPTH)]


def _prog(name):
    if name not in _PROGS:
        _PROGS[name] = {'A': build_A, 'B': build_B, 'C': build_C, 'M': build_M}[name]()
    return _PROGS[name]


def _run(name, ims):
    import sys as _sys
    import time as _time
    t0 = _time.time()
    prog = _prog(name)
    t1 = _time.time()
    res = run_bass_kernel_spmd(prog, ims, core_ids=list(range(N_CORES)))
    print('[kernel] launch %s build %.1fs run %.1fs' % (name, t1 - t0, _time.time() - t1), file=_sys.stderr, flush=True)
    return res.results


def run_M(W):
    cT = np.ascontiguousarray(_pk(np.stack([W['c'][0], W['c'][1], W['c_ctx']])).transpose(0, 2, 1))
    lq = np.ascontiguousarray(np.stack([W['lambda_q1'], W['lambda_k1'], W['lambda_q2'], W['lambda_k2']], axis=1))
    li = np.array(LAM_INIT, np.float32).reshape(4, 1)
    ims = []
    for i in range(N_CORES):
        l, hf = i // 2, i % 2
        ims.append({'cT': cT, 'w': np.ascontiguousarray(W['w_ada'][l][:, hf * MH:(hf + 1) * MH]),
                    'b': np.ascontiguousarray(W['b_ada'][l][None, hf * MH:(hf + 1) * MH]), 'lq': lq, 'li': li})
    res = _run('M', ims)
    mods = np.zeros((DEPTH, 3, 6, D), np.float32)
    for l in range(DEPTH):
        full = np.concatenate([np.asarray(res[2 * l]['modo']), np.asarray(res[2 * l + 1]['modo'])], axis=1)
        mods[l] = full.reshape(3, 6, D)
    lam = np.asarray(res[0]['lamo']).reshape(4).astype(np.float32)
    return mods, lam


def _halo_fm(xbT, s, xcbT):
    out = np.zeros((D, NT), np.float32)
    lo = s * TOK - HALO
    hi = (s + 1) * TOK + HALO
    a, b = max(lo, 0), min(hi, SEQ)
    out[:, a - lo:a - lo + (b - a)] = xbT[:, a:b]
    out[:, CTX0:CTX0 + CTX] = xcbT
    return out


def _mod_in(mods, l, b):
    return np.ascontiguousarray(np.stack([_pk(mods[l, b]), _pk(mods[l, 2])], axis=1))


def run_A(l, xT, xcT, mods, W):
    ims = []
    for i in range(N_CORES):
        b, s = i // 4, i % 4
        cosT, sinT = _rope_tables(s)
        ims.append({
            'xT': _halo_fm(xT[b], s, xcT[b]), 'mod': _mod_in(mods, l, b), 'g1': _pk(W['norm1_g'][l]), 'w_in': W['w_in'][l],
            'cosT': cosT, 'sinT': sinT, 'perm': _perm(), 'invc': _invcnt(s),
            'valid': np.tile(np.array([[1.0 if s > 0 else 0.0, 1.0 if s < 3 else 0.0]], np.float32), (128, 1)),
            'pool_w': W['pool_w'][l], 'pool_s': np.ascontiguousarray(W['pool_scale'][l].reshape(4, 128).T),
            'conv_w': np.ascontiguousarray(W['conv_w'][l].reshape(3, 4, 128).transpose(2, 1, 0)),
        })
    return _run('A', ims)


def _own_fm(xT, xcT, i):
    b, s = i // 4, i % 4
    return np.ascontiguousarray(np.concatenate([xT[b][:, s * TOK:(s + 1) * TOK], xcT[b]], axis=1))


def run_B(l, resA, xT, xcT, mods, lam, W):
    kTs, vhs = [], []
    for b in range(2):
        qk = [np.asarray(resA[4 * b + s]['qkvT']) for s in range(4)]
        kT = np.concatenate([qk[0][1024:2048, TOK:NO]] + [qk[s][1024:2048, 0:TOK] for s in range(4)], axis=1)
        vT = np.concatenate([qk[0][2048:3072, TOK:NO]] + [qk[s][2048:3072, 0:TOK] for s in range(4)], axis=1)
        v = vT.T
        vh = np.ascontiguousarray(v.reshape(KT, 128, 8, 128).transpose(2, 1, 0, 3).reshape(8, 128, NK))
        kTs.append(np.ascontiguousarray(kT))
        vhs.append(vh)
    sc = np.zeros((128, 3), np.float32)
    sc[:, 0] = lam[l]
    sc[:, 1] = W['subln_g'][l]
    sc[:, 2] = np.float32(1.0 - LAM_INIT[l])
    w_r = np.ascontiguousarray(W['w_router'][l].reshape(16, 128, 16).transpose(1, 0, 2))
    ims = []
    for i in range(N_CORES):
        b = i // 4
        ims.append({'qT': np.ascontiguousarray(np.asarray(resA[i]['qkvT'])[0:1024]), 'kT': kTs[b], 'vh': vhs[b],
                    'mixT': np.asarray(resA[i]['mixT']), 'xT': _own_fm(xT, xcT, i), 'w_out': W['w_out'][l],
                    'mod': _mod_in(mods, l, b), 'g2': _pk(W['norm2_g'][l]), 'sc': sc, 'w_r': w_r})
    return _run('B', ims)


def run_C(l, resB, mods, W):
    sel = np.zeros((16, 16, 128), np.float32)
    for e in range(16):
        sel[e, e, :] = 1.0
    logAs = []
    for b in range(2):
        lg = [np.asarray(resB[4 * b + s]['logT']) for s in range(4)]
        logAs.append(np.ascontiguousarray(np.concatenate([lg[s][:, 0:TOK] for s in range(4)] + [lg[0][:, TOK:NO]], axis=1)))
    ims = []
    for i in range(N_CORES):
        b = i // 4
        ims.append({'logA': logAs[b], 'logO': np.asarray(resB[i]['logT']), 'h2T': np.asarray(resB[i]['h2T']),
                    'xmidT': np.asarray(resB[i]['xmidT']), 'mod': _mod_in(mods, l, b), 'fg': _pk(W['final_g']), 'sel': sel,
                    'w_gate': W['w_gate'][l], 'w_up': W['w_up'][l], 'w_down': W['w_down'][l]})
    return _run('C', ims)


def kernel(**inputs):
    W = {k: np.asarray(v, dtype=np.float32) for k, v in inputs.items()}
    mods, lam = run_M(W)
    xT = [np.ascontiguousarray(W['x'][b].T) for b in range(2)]
    xcT = [np.ascontiguousarray(W['ctx'][b].T) for b in range(2)]
    out = np.zeros((2, SEQ, D), np.float32)
    for l in range(DEPTH):
        resA = run_A(l, xT, xcT, mods, W)
        resB = run_B(l, resA, xT, xcT, mods, lam, W)
        del resA
        resC = run_C(l, resB, mods, W)
        del resB
        for i in range(N_CORES):
            b, s = i // 4, i % 4
            xo = np.asarray(resC[i]['xoutT'])
            xT[b][:, s * TOK:(s + 1) * TOK] = xo[:, 0:TOK]
            if s == 0:
                xcT[b] = np.ascontiguousarray(xo[:, TOK:NO])
            if l == DEPTH - 1:
                out[b, s * TOK:(s + 1) * TOK, :] = np.asarray(resC[i]['xnT'])[:, 0:TOK].T
        del resC
    return out
```
